# Optimizing a Trainium2 kernel written in Bass

```python
import jax, jax.numpy as jnp
from jax import lax
import numpy as np

D_MODEL = 2048
BATCH = 4
SEQ = 2048
DEPTH = 4

MEM_LEN = 256
D_FF = 5632
CONV_WIDTH = 1024
CONV_K = 3
HGRN_HEADS = 8
HGRN_DK = 128
HGRN_DV = 128
HGRN_KW = HGRN_HEADS * HGRN_DK
HGRN_VW = HGRN_HEADS * HGRN_DV
HGRN_CHUNK = 64
XATTN_HEADS = 4
XATTN_DH = 256
XATTN_WIDTH = XATTN_HEADS * XATTN_DH
N_BRANCH = 3
BRANCH_WIDTH = 1024
EPS = 1e-6
F_FLOOR = 1e-30
IN_WIDTHS = (CONV_WIDTH, CONV_WIDTH, CONV_WIDTH,
             HGRN_KW, HGRN_KW, HGRN_VW, HGRN_VW,
             XATTN_WIDTH,
             N_BRANCH * D_MODEL)
IN_COLS = sum(IN_WIDTHS)
IN_SPLITS = tuple(int(v) for v in np.cumsum(IN_WIDTHS)[:-1])

kernel_name = "hybrid_gated_conv_hgrn2_memxattn_macaron"


def rmsnorm(x, g):
    xf = x.astype(jnp.float32)
    y = xf * lax.rsqrt(jnp.mean(xf * xf, axis=-1, keepdims=True) + EPS) * g.astype(jnp.float32)
    return y.astype(x.dtype)


def swiglu(h, w_gate, w_up, w_down):
    return (jax.nn.silu(h @ w_gate) * (h @ w_up)) @ w_down


def short_gated_conv(x_in, b_gate, c_gate, conv_w):
    u = c_gate * x_in
    seq = u.shape[1]
    u_pad = jnp.pad(u, ((0, 0), (CONV_K - 1, 0), (0, 0)))
    y = sum(conv_w[j] * u_pad[:, j:j + seq] for j in range(CONV_K))
    return b_gate * y


def hgrn2_chunked(q, k, v, log_f):
    bsz, seq = q.shape[0], q.shape[1]
    n_chunks = seq // HGRN_CHUNK

    def to_chunks(t):
        return t.reshape(bsz, n_chunks, HGRN_CHUNK, HGRN_HEADS, -1).transpose(1, 0, 3, 2, 4)

    causal = jnp.tril(jnp.ones((HGRN_CHUNK, HGRN_CHUNK), dtype=bool))[:, :, None]

    def step(state, inp):
        qc, kc, vc, lfc = inp
        b = jnp.cumsum(lfc, axis=2)
        diff = b[:, :, :, None, :] - b[:, :, None, :, :]
        decay = jnp.where(causal, jnp.exp(jnp.where(causal, diff, 0.0)), 0.0)
        scores = jnp.einsum('bhtd,bhsd,bhtsd->bhts', qc, kc, decay)
        o = (jnp.einsum('bhts,bhsv->bhtv', scores, vc)
             + jnp.einsum('bhtd,bhdv->bhtv', qc * jnp.exp(b), state))
        b_last = b[:, :, -1, :]
        state = (jnp.exp(b_last)[..., None] * state
                 + jnp.einsum('bhsd,bhsv->bhdv', kc * jnp.exp(b_last[:, :, None, :] - b), vc))
        return state, o

    s0 = jnp.zeros((bsz, HGRN_HEADS, HGRN_DK, HGRN_DV), jnp.float32)
    _, o = lax.scan(step, s0, (to_chunks(q), to_chunks(k), to_chunks(v), to_chunks(log_f)))
    return o.transpose(1, 0, 3, 2, 4).reshape(bsz, seq, HGRN_HEADS, HGRN_DV)


def hgrn2_branch(q, z, i, g, lb, norm_g):
    bsz, seq = q.shape[0], q.shape[1]
    shp_k = (bsz, seq, HGRN_HEADS, HGRN_DK)
    qf = q.astype(jnp.float32).reshape(shp_k)
    zf = z.astype(jnp.float32).reshape(shp_k)
    vf = i.astype(jnp.float32).reshape(bsz, seq, HGRN_HEADS, HGRN_DV)
    lbh = lb.astype(jnp.float32).reshape(HGRN_HEADS, HGRN_DK)
    f = lbh + (1.0 - lbh) * jax.nn.sigmoid(zf)
    log_f = jnp.log(jnp.maximum(f, F_FLOOR))
    kf = (1.0 - lbh) * jax.nn.sigmoid(-zf)
    o = hgrn2_chunked(qf, kf, vf, log_f)
    o = rmsnorm(o, norm_g).reshape(bsz, seq, HGRN_VW)
    return o.astype(g.dtype) * jax.nn.silu(g)


def memory_cross_attention(q, mem_n, w_mem_kv):
    bsz, seq = q.shape[0], q.shape[1]
    kv = mem_n @ w_mem_kv
    k, v = jnp.split(kv, 2, axis=-1)
    qh = q.reshape(bsz, seq, XATTN_HEADS, XATTN_DH)
    kh = k.reshape(bsz, -1, XATTN_HEADS, XATTN_DH)
    vh = v.reshape(bsz, -1, XATTN_HEADS, XATTN_DH)
    scores = jnp.einsum('bshd,bmhd->bhsm', qh, kh).astype(jnp.float32) * (XATTN_DH ** -0.5)
    probs = jax.nn.softmax(scores, axis=-1).astype(vh.dtype)
    out = jnp.einsum('bhsm,bmhd->bshd', probs, vh)
    return out.reshape(bsz, seq, XATTN_WIDTH)


def setup_inputs(seed: int = 0) -> dict:
    key = jax.random.key(seed)
    ks = jax.random.split(key, 24)

    def nrm(k, shape, scale):
        return jax.random.normal(k, shape, jnp.float32) * scale

    def gain(k, shape):
        return 1.0 + 0.02 * jax.random.normal(k, shape, jnp.float32)

    L, D, F = DEPTH, D_MODEL, D_FF
    return {
        "x": nrm(ks[0], (BATCH, SEQ, D), 1.0),
        "mem": nrm(ks[1], (BATCH, MEM_LEN, D), 1.0),
        "norm_ffn1": gain(ks[2], (L, D)),
        "ffn1_w_gate": nrm(ks[3], (L, D, F), D ** -0.5),
        "ffn1_w_up": nrm(ks[4], (L, D, F), D ** -0.5),
        "ffn1_w_down": nrm(ks[5], (L, F, D), F ** -0.5),
        "norm_mix": gain(ks[6], (L, D)),
        "w_in": nrm(ks[7], (L, D, IN_COLS), D ** -0.5),
        "conv_w": nrm(ks[8], (L, CONV_K, CONV_WIDTH), CONV_K ** -0.5),
        "hgrn_lb_logits": nrm(ks[9], (L, HGRN_KW), 0.1),
        "hgrn_norm": gain(ks[10], (L, HGRN_DV)),
        "mem_norm": gain(ks[11], (L, D)),
        "w_mem_kv": nrm(ks[12], (L, D, 2 * XATTN_WIDTH), D ** -0.5),
        "w_branch": nrm(ks[13], (L, N_BRANCH, BRANCH_WIDTH, D), BRANCH_WIDTH ** -0.5),
        "w_o": nrm(ks[14], (L, D, D), D ** -0.5),
        "norm_ffn2": gain(ks[15], (L, D)),
        "ffn2_w_gate": nrm(ks[16], (L, D, F), D ** -0.5),
        "ffn2_w_up": nrm(ks[17], (L, D, F), D ** -0.5),
        "ffn2_w_down": nrm(ks[18], (L, F, D), F ** -0.5),
        "final_norm": gain(ks[19], (D,)),
    }


def reference(x, mem, norm_ffn1, ffn1_w_gate, ffn1_w_up, ffn1_w_down, norm_mix, w_in, conv_w,
              hgrn_lb_logits, hgrn_norm, mem_norm, w_mem_kv, w_branch, w_o, norm_ffn2,
              ffn2_w_gate, ffn2_w_up, ffn2_w_down, final_norm):
    bsz, seq = x.shape[0], x.shape[1]
    p = jax.nn.softmax(hgrn_lb_logits.astype(jnp.float32), axis=0)
    lower_bounds = jnp.cumsum(p, axis=0) - p[0]

    for l in range(DEPTH):
        x = x + 0.5 * swiglu(rmsnorm(x, norm_ffn1[l]), ffn1_w_gate[l], ffn1_w_up[l], ffn1_w_down[l])

        h = rmsnorm(x, norm_mix[l])
        (c_x, c_b, c_c, h_q, h_f, h_i, h_g, a_q, gate_logits) = jnp.split(h @ w_in[l], IN_SPLITS, axis=-1)
        y_conv = short_gated_conv(c_x, c_b, c_c, conv_w[l])
        y_hgrn = hgrn2_branch(h_q, h_f, h_i, h_g, lower_bounds[l], hgrn_norm[l])
        y_mem = memory_cross_attention(a_q, rmsnorm(mem, mem_norm[l]), w_mem_kv[l])

        ys = jnp.stack([y_conv, y_hgrn, y_mem], axis=2)
        proj = jnp.einsum('bsnc,ncd->bsnd', ys, w_branch[l])
        gates = jax.nn.sigmoid(gate_logits.reshape(bsz, seq, N_BRANCH, D_MODEL))
        merged = jnp.sum(gates * proj, axis=2)
        x = x + merged @ w_o[l]

        x = x + 0.5 * swiglu(rmsnorm(x, norm_ffn2[l]), ffn2_w_gate[l], ffn2_w_up[l], ffn2_w_down[l])

    return rmsnorm(x, final_norm)
```

```python
import numpy as np
import ml_dtypes
import concourse.bass as bass
import concourse.mybir as mybir
from concourse.bass_utils import run_bass_kernel_spmd

F32 = mybir.dt.float32
BF16 = mybir.dt.bfloat16
AF = mybir.ActivationFunctionType
ALU = mybir.AluOpType

L = 4
D = 2048
T = 1024
KC = 16
FF = 5632
NF = 44
GJ = 11
NG = 4
EPS = 1e-6
MIXCUT = 99
CONVDBG = 0
NSLOT = 3
SLOT = 4096

PO = {}
_o = 0
for _n, _w in [("n1", L * 16), ("nm", L * 16), ("n2", L * 16), ("nmem", L * 16), ("nf", 16),
               ("cw", L * 3 * 8), ("lbl", L * 8), ("hn", L), ("flag", 1)]:
    PO[_n] = _o
    _o += _w
NPAR = _o


class Buf:
    __slots__ = ("w", "r")

    def __init__(self):
        self.w = None
        self.r = []


class TB:
    def __init__(self, ap):
        self.buf = Buf()
        self.ap = ap


class Eng:
    def __init__(self, name):
        self.name = name
        self.ops = []
        self.cnt = 0
        self.seen = {}


class Prog:
    def __init__(self):
        self.E = {n: Eng(n) for n in ["pe", "act", "dve", "pool", "sp"]}
        self.dcnt = {}

    def _deps(self, eng, reads, writes, extra):
        toks = list(extra)
        for b in reads:
            if b.w is not None:
                toks.append(b.w)
        for b in writes:
            if b.w is not None:
                toks.append(b.w)
            toks.extend(b.r)
        for (k, v) in toks:
            if eng.seen.get(k, 0) >= v:
                continue
            eng.seen[k] = v
            eng.ops.append(("wait", k, v))

    def _commit(self, tok, reads, writes):
        for b in reads:
            b.r.append(tok)
        for b in writes:
            b.w = tok
            b.r = []

    def op(self, en, fn, reads=(), writes=(), extra=()):
        return self.group(en, [fn], reads, writes, extra)

    def group(self, en, fns, reads=(), writes=(), extra=()):
        eng = self.E[en]
        reads = [b.buf if isinstance(b, TB) else b for b in reads]
        writes = [b.buf if isinstance(b, TB) else b for b in writes]
        self._deps(eng, reads, writes, extra)
        for fn in fns[:-1]:
            eng.ops.append(("op", fn, False))
        eng.cnt += 1
        tok = (en, eng.cnt)
        eng.ops.append(("op", fns[-1], True))
        self._commit(tok, reads, writes)
        return tok

    def dma(self, en, fn, semkey, reads=(), writes=(), extra=()):
        eng = self.E[en]
        reads = [b.buf if isinstance(b, TB) else b for b in reads]
        writes = [b.buf if isinstance(b, TB) else b for b in writes]
        self._deps(eng, reads, writes, extra)
        self.dcnt[semkey] = self.dcnt.get(semkey, 0) + 16
        tok = (semkey, self.dcnt[semkey])
        eng.ops.append(("dma", fn, semkey))
        self._commit(tok, reads, writes)
        return tok

    def custom(self, en, fn, semkey, inc, reads=(), writes=(), extra=()):
        eng = self.E[en]
        reads = [b.buf if isinstance(b, TB) else b for b in reads]
        writes = [b.buf if isinstance(b, TB) else b for b in writes]
        self._deps(eng, reads, writes, extra)
        self.dcnt[semkey] = self.dcnt.get(semkey, 0) + inc
        tok = (semkey, self.dcnt[semkey])
        eng.ops.append(("custom", fn, semkey, inc))
        self._commit(tok, reads, writes)
        return tok

    def wait(self, en, toks):
        self._deps(self.E[en], [], [], [t for t in toks if t is not None])

    def barrier(self, engs=("pe", "act", "dve"), extra=()):
        toks = [(e, self.E[e].cnt) for e in engs if self.E[e].cnt > 0] + [t for t in extra if t is not None]
        for e in engs:
            if e == "pe":
                continue
            self._deps(self.E[e], [], [], toks)


def build(stages, final_norm=True, ncores=8):
    nc = bass.Bass("TRN2", target_bir_lowering=False)
    P = Prog()
    layers = sorted({l for _, l in stages})
    NL = len(layers)
    LI = {l: i for i, l in enumerate(layers)}

    def din(name, shape, dt=F32):
        return nc.dram_tensor(name, list(shape), dt, kind="ExternalInput").ap()

    x_d = din("xT", [D, T])
    mem_d = din("memT", [D, 256])
    par_d = din("par", [128, NPAR])
    cst_d = din("cst", [128, 2048])
    y_d = nc.dram_tensor("yT", [D, T], F32, kind="ExternalOutput").ap()
    xsp_d = nc.dram_tensor("xspill", [D, T], F32, kind="Internal").ap()
    ccs_d = nc.dram_tensor("cc_src", [128, 1152], F32, kind="Internal").ap()
    ccd_d = nc.dram_tensor("cc_dst", [256, 1152], F32, kind="Internal").ap()
    WSH = {"gu1": [NL * NF, 128, 4096], "gu2": [NL * NF, 128, 4096], "d1": [NL * NG * 8, 128, 2816],
           "d2": [NL * NG * 8, 128, 2816], "qf": [NL * 8, 128, 4096], "vi": [NL * 4, 128, 4096],
           "hg": [NL * 8, 128, 2048], "cxc": [NL * 8, 128, 4096], "cb": [NL * 8, 128, 2048],
           "aq": [NL * 4, 128, 4096], "mg": [NL * 48, 128, 3072], "wo": [NL * 8, 128, 4096],
           "kk": [NL * 4, 128, 4096], "kv": [NL * 4, 128, 4096]}
    W = {}

    def getW(fam):
        if fam not in W:
            W[fam] = din("w_" + fam, WSH[fam])
        return W[fam]

    import contextlib
    es = contextlib.ExitStack()
    with es:
        def sb(name, shape, dt):
            return es.enter_context(nc.sbuf_tensor(name, list(shape), dt))

        hT_t = sb("hT", [128, KC * T], BF16)
        slots_t = sb("slots", [128, NSLOT * SLOT], BF16)
        par_t = sb("par_s", [128, NPAR], F32)
        cstf_t = sb("cstf", [128, 2048], F32)
        cstb_t = sb("cstb", [128, 1152], BF16)
        lb_t = sb("lb", [128, 2 * L * 8 + 16], F32)
        X_t = sb("X", [128, 16384], F32)
        R_t = sb("R", [128, 18432], F32)
        ps_t = es.enter_context(nc.psum_tensor("ps", [128, 4096], F32))

        hT = TB(hT_t[:, :].rearrange("p (c t) -> p c t", c=KC))
        xT = TB(X_t[:, :].rearrange("p (c t) -> p c t", c=KC))
        par = TB(par_t[:, :])
        cstf = cstf_t[:, :]
        ident = cstf[:, 0:128]
        rmask = cstf[:, 128:1152]
        cmask = cstf[:, 1152:1216]
        ones_b = cstb_t[:, 0:128]
        CST = Buf()
        pairs = [TB(ps_t[:, k * 1024:(k + 1) * 1024]) for k in range(4)]
        pstate = {"i": 0}

        def palloc():
            p = pairs[pstate["i"] % 4]
            pstate["i"] += 1
            return p

        slot_bufs = [TB(slots_t[:, k * SLOT:(k + 1) * SLOT]) for k in range(NSLOT)]
        sstate = {"i": 0}

        def wload(fam, idx, n):
            s = slot_bufs[sstate["i"] % NSLOT]
            key = "ws%d" % (sstate["i"] % NSLOT)
            sstate["i"] += 1
            src = getW(fam)[idx]
            dst = s.ap[:, 0:n]
            P.dma("pool", lambda g, dst=dst, src=src: g.dma_start(out=dst, in_=src), key, reads=[], writes=[s])
            return s

        def rbytes_view(tensor, off_f32, n_elems, dt):
            if dt == F32:
                return tensor[:, off_f32:off_f32 + n_elems]
            assert n_elems % 2 == 0
            return tensor[:, off_f32:off_f32 + n_elems // 2].bitcast(BF16)

        class Arena:
            def __init__(self, tensor, base, size):
                self.t = tensor
                self.base = base
                self.size = size
                self.off = 0

            def reset(self):
                self.off = 0

            def get(self, n_elems, dt, init_r=()):
                words = n_elems if dt == F32 else n_elems // 2
                assert self.off + words <= self.size, ("arena overflow", self.off, words, self.size)
                v = rbytes_view(self.t, self.base + self.off, n_elems, dt)
                self.off += words
                tb = TB(v)
                tb.buf.r = list(init_r)
                return tb

        RA = Arena(R_t, 0, 18432)
        RB = Arena(R_t, 12288, 6144)
        RY = [Arena(R_t, 4096 * i, 4096) for i in range(3)]
        XA = Arena(X_t, 0, 16384)

        P.dma("sp", lambda e: e.dma_start(out=par_t[:, :], in_=par_d[:, :]), "ld_par", writes=[par])
        P.dma("sp", lambda e: e.dma_start(out=cstf_t[:, :], in_=cst_d[:, :]), "ld_cst", writes=[CST])
        for q in range(4):
            P.dma("sp", lambda e, q=q: e.dma_start(
                out=xT.ap[:, q * 4:(q + 1) * 4, :],
                in_=x_d[q * 512:(q + 1) * 512, :].rearrange("(c p) t -> p c t", p=128)),
                "ld_x%d" % q, writes=[xT])
        P.op("dve", lambda v: v.memset(cstb_t[:, 0:128], 1.0), writes=[CST])
        lbl = par_t[:, PO["lbl"]:PO["lbl"] + L * 8].rearrange("p (l h) -> p l h", l=L)
        ex_t = lb_t[:, 0:L * 8].rearrange("p (l h) -> p l h", l=L)
        LB = TB(lb_t[:, :])
        oml_v = lb_t[:, L * 8:2 * L * 8].rearrange("p (l h) -> p l h", l=L)
        ssum = lb_t[:, 2 * L * 8:2 * L * 8 + 8]
        P.op("act", lambda a: a.activation(out=ex_t, in_=lbl, func=AF.Exp), reads=[par], writes=[LB])
        P.op("dve", lambda v: v.tensor_tensor(out=ssum, in0=ex_t[:, 0, :], in1=ex_t[:, 1, :], op=ALU.add), reads=[LB], writes=[LB])
        P.op("dve", lambda v: v.tensor_tensor(out=ssum, in0=ssum, in1=ex_t[:, 2, :], op=ALU.add), reads=[LB], writes=[LB])
        P.op("dve", lambda v: v.tensor_tensor(out=ssum, in0=ssum, in1=ex_t[:, 3, :], op=ALU.add), reads=[LB], writes=[LB])
        P.op("dve", lambda v: v.reciprocal(out=ssum, in_=ssum), reads=[LB], writes=[LB])
        for l in range(L):
            P.op("dve", lambda v, l=l: v.tensor_tensor(out=ex_t[:, l, :], in0=ex_t[:, l, :], in1=ssum, op=ALU.mult), reads=[LB], writes=[LB])
        P.op("dve", lambda v: v.tensor_tensor(out=ex_t[:, 3, :], in0=ex_t[:, 3, :], in1=ex_t[:, 2, :], op=ALU.add), reads=[LB], writes=[LB])
        P.op("dve", lambda v: v.tensor_tensor(out=ex_t[:, 3, :], in0=ex_t[:, 3, :], in1=ex_t[:, 1, :], op=ALU.add), reads=[LB], writes=[LB])
        P.op("dve", lambda v: v.tensor_tensor(out=ex_t[:, 2, :], in0=ex_t[:, 2, :], in1=ex_t[:, 1, :], op=ALU.add), reads=[LB], writes=[LB])
        P.op("dve", lambda v: v.memset(ex_t[:, 0, :], 0.0), reads=[LB], writes=[LB])
        P.op("dve", lambda v: v.tensor_scalar(out=oml_v, in0=ex_t, scalar1=-1.0, scalar2=1.0, op0=ALU.mult, op1=ALU.add), reads=[LB], writes=[LB])
        lb_v = ex_t

        def pcol(name, i):
            return par_t[:, PO[name] + i:PO[name] + i + 1]

        def rms_stats(src_ap_fn, src_buf, nchunks, ntok, inv_n, arena):
            pair = palloc()
            sq = [arena.get(ntok, BF16) for _ in range(2)]
            nt = (ntok + 511) // 512
            w = min(ntok, 512)
            for c in range(nchunks):
                s = sq[c % 2]
                if c % 2 == 0:
                    P.op("act", lambda a, c=c, s=s: a.activation(out=s.ap, in_=src_ap_fn(c), func=AF.Square),
                         reads=[src_buf], writes=[s])
                else:
                    P.op("dve", lambda v, c=c, s=s: v.tensor_tensor(out=s.ap, in0=src_ap_fn(c), in1=src_ap_fn(c), op=ALU.mult),
                         reads=[src_buf], writes=[s])
                P.group("pe", [
                    (lambda t, c=c, s=s, tt=tt: t.matmul(pair.ap[:, tt * 512:tt * 512 + w], lhsT=ones_b,
                                                        rhs=s.ap[:, tt * w:(tt + 1) * w],
                                                        start=(c == 0), stop=(c == nchunks - 1)))
                    for tt in range(nt)], reads=[s, CST], writes=[pair])
            rstd = arena.get(ntok, F32)
            if nt == 2:
                pv = pair.ap[:, 0:ntok]
            else:
                pv = pair.ap[:, 0:w]
            P.op("act", lambda a: a.activation(out=rstd.ap, in_=pv, func=AF.Ln, bias=epsb, scale=inv_n),
                 reads=[pair, CST], writes=[rstd])
            P.op("act", lambda a: a.activation(out=rstd.ap, in_=rstd.ap, func=AF.Exp, scale=-0.5),
                 reads=[rstd], writes=[rstd])
            return rstd

        epsb = lb_t[:, 2 * L * 8 + 8:2 * L * 8 + 9]
        P.op("dve", lambda v: v.memset(epsb, EPS), writes=[CST])

        def norm_to_hT(gname, l, arena):
            rstd = rms_stats(lambda c: xT.ap[:, c, :], xT, KC, T, 1.0 / D, arena)
            for c in range(KC):
                P.op("dve", lambda v, c=c: v.scalar_tensor_tensor(
                    out=hT.ap[:, c, :], in0=xT.ap[:, c, :], scalar=pcol(gname, l * 16 + c), in1=rstd.ap,
                    op0=ALU.mult, op1=ALU.mult), reads=[xT, rstd, par], writes=[hT])

        def ffn(l, which):
            RA.reset()
            P.barrier()
            norm_to_hT("n1" if which == 1 else "n2", l, RA)
            hid = RA.get(GJ * T, BF16)
            hv = hid.ap.rearrange("p (j t) -> p j t", j=GJ)
            sg = [RA.get(T, F32) for _ in range(2)]
            gu = "gu%d" % which
            dn = "d%d" % which
            for g in range(NG):
                for j in range(GJ):
                    f = g * GJ + j
                    s = wload(gu, LI[l] * NF + f, 4096)
                    wv = s.ap.rearrange("p (g k m) -> p g k m", g=2, k=KC)
                    pp = []
                    for q in range(2):
                        pair = palloc()
                        pp.append(pair)
                        P.group("pe", [
                            (lambda t, q=q, kc=kc, tt=tt, pair=pair, wv=wv: t.matmul(
                                pair.ap[:, tt * 512:(tt + 1) * 512], lhsT=wv[:, q, kc, :],
                                rhs=hT.ap[:, kc, tt * 512:(tt + 1) * 512], start=(kc == 0), stop=(kc == KC - 1)))
                            for kc in range(KC) for tt in range(2)], reads=[s, hT], writes=[pair])
                    st = sg[f % 2]
                    P.op("act", lambda a, st=st, pg=pp[0]: a.activation(out=st.ap, in_=pg.ap, func=AF.Silu),
                         reads=[pp[0]], writes=[st])
                    P.op("dve", lambda v, st=st, pu=pp[1], j=j: v.tensor_tensor(
                        out=hv[:, j, :], in0=st.ap, in1=pu.ap, op=ALU.mult), reads=[st, pp[1]], writes=[hid])
                for dcp in range(8):
                    s = wload(dn, (LI[l] * NG + g) * 8 + dcp, 2816)
                    wv = s.ap[:, 0:2816].rearrange("p (d j m) -> p d j m", d=2, j=GJ)
                    for d2 in range(2):
                        dc = dcp * 2 + d2
                        pair = palloc()
                        P.group("pe", [
                            (lambda t, d2=d2, j=j, tt=tt, pair=pair, wv=wv: t.matmul(
                                pair.ap[:, tt * 512:(tt + 1) * 512], lhsT=wv[:, d2, j, :],
                                rhs=hv[:, j, tt * 512:(tt + 1) * 512], start=(j == 0), stop=(j == GJ - 1)))
                            for j in range(GJ) for tt in range(2)], reads=[s, hid], writes=[pair])
                        P.op("dve", lambda v, dc=dc, pair=pair: v.scalar_tensor_tensor(
                            out=xT.ap[:, dc, :], in0=pair.ap, scalar=0.5, in1=xT.ap[:, dc, :],
                            op0=ALU.mult, op1=ALU.add), reads=[pair, xT], writes=[xT])

        cmask8 = cstf[:, 1152:1664].rearrange("p (b t) -> p b t", b=8)
        ones16 = cstf[:, 1664:1680]
        rg_pairs = [[2 * i, 2 * i + 1] for i in range(ncores // 2)]

        def mm32(pair, lhs_fn, reads):
            return P.group("pe", [
                (lambda t, kc=kc, tt=tt: t.matmul(pair.ap[:, tt * 512:(tt + 1) * 512], lhsT=lhs_fn(kc),
                                                  rhs=hT.ap[:, kc, tt * 512:(tt + 1) * 512],
                                                  start=(kc == 0), stop=(kc == KC - 1)))
                for kc in range(KC) for tt in range(2)], reads=list(reads) + [hT], writes=[pair])

        def mixer(l):
            li = LI[l]
            RA.reset()
            P.barrier()
            norm_to_hT("nm", l, RA)
            spill = []
            for q in range(4):
                spill.append(P.dma("sp", lambda e, q=q: e.dma_start(
                    out=xsp_d[q * 512:(q + 1) * 512, :].rearrange("(c p) t -> p c t", p=128),
                    in_=xT.ap[:, q * 4:(q + 1) * 4, :]), "sp_x%d" % q, reads=[xT]))
            P.barrier()

            def reload_x():
                P.barrier()
                bt = [(e, P.E[e].cnt) for e in ("pe", "act", "dve")]
                for q in range(4):
                    P.dma("sp", lambda e, q=q: e.dma_start(
                        out=xT.ap[:, q * 4:(q + 1) * 4, :],
                        in_=xsp_d[q * 512:(q + 1) * 512, :].rearrange("(c p) t -> p c t", p=128)),
                        "ld_x%d" % q, writes=[xT], extra=bt + spill)
            if MIXCUT == 0:
                reload_x()
                return
            RA.reset()
            XA.reset()
            Vt = RA.get(8192, BF16)
            Vv = Vt.ap.rearrange("p (b c) -> p b c", b=8)
            A, B, C, Dd, E, Q = [RA.get(1024, F32) for _ in range(6)]
            Qt, Kb, Qh = [RA.get(1024, BF16) for _ in range(3)]
            Kp = RA.get(2048, BF16)
            Kp4 = Kp.ap.rearrange("p (b two d) -> p b two d", two=2, d=128)
            Kp3 = Kp.ap.rearrange("p (c d) -> p c d", d=128)
            Sm = RA.get(1024, BF16)
            Sm4 = Sm.ap.rearrange("p (b two t) -> p b two t", two=2, t=64)
            Sm3 = Sm.ap.rearrange("p (c t) -> p c t", t=64)
            Sall = RA.get(2048, F32)
            Sall3 = Sall.ap.rearrange("p (c d) -> p c d", d=128)
            Sb = RA.get(2048, BF16)
            Sb3 = Sb.ap.rearrange("p (c d) -> p c d", d=128)
            sm = RA.get(128, F32)
            dLm, eLm, aC, BcI, Bx = [sm.ap[:, i * 16:(i + 1) * 16] for i in range(5)]
            Qff = XA.get(8192, BF16, spill)
            Qff3 = Qff.ap.rearrange("p (h t) -> p h t", h=8)
            oloc = XA.get(8192, F32, spill)
            oloc3 = oloc.ap.rearrange("p (h t) -> p h t", h=8)
            sendb = XA.get(1152, F32, spill)
            recvb = XA.get(1152, F32, spill)
            Sin = XA.get(1024, BF16, spill)
            Sin3 = Sin.ap.rearrange("p (h d) -> p h d", h=8)
            xsm = XA.get(640, F32, spill)
            uprev = xsm.ap[:, 0:128]
            cpre = xsm.ap[:, 128:256]
            cbs = xsm.ap[:, 256:384]
            t1s = xsm.ap[:, 384:400]
            t2s = xsm.ap[:, 400:416]
            yfix = xsm.ap[:, 416:432]
            mask10 = cstf[:, 1680:1696].rearrange("p (j k) -> p j k", k=2)
            P.op("dve", lambda v: v.memset(Kp.ap, 0.0), writes=[Kp])
            P.op("dve", lambda v: v.memset(Sm.ap, 0.0), writes=[Sm])
            for hp in range(4):
                s = wload("vi", li * 4 + hp, 4096)
                wv = s.ap.rearrange("p (k n) -> p k n", k=KC)
                for tbp in range(2):
                    pair = palloc()
                    P.group("pe", [
                        (lambda t, tbi=tbi, kc=kc, pair=pair, wv=wv, tbp=tbp: t.matmul(
                            pair.ap[:, tbi * 256:(tbi + 1) * 256],
                            lhsT=hT.ap[:, kc, (tbp * 4 + tbi) * 128:(tbp * 4 + tbi + 1) * 128],
                            rhs=wv[:, kc, :], start=(kc == 0), stop=(kc == KC - 1)))
                        for tbi in range(4) for kc in range(KC)], reads=[s, hT], writes=[pair])
                    P.op("act", lambda a, pair=pair, tbp=tbp, hp=hp: a.activation(
                        out=Vv[:, tbp * 4:(tbp + 1) * 4, hp * 256:(hp + 1) * 256],
                        in_=pair.ap.rearrange("p (b n) -> p b n", b=4), func=AF.Copy), reads=[pair], writes=[Vt])
            def halves(tb):
                return [TB(tb.ap[:, 0:512]), TB(tb.ap[:, 512:1024])]

            def v3(tb):
                return tb.ap.rearrange("p (c t) -> p c t", t=64)

            Ah, Bh, Ch, Dh, Eh, Qs, Qth, Kbh, Qhh = [halves(x_) for x_ in (A, B, C, Dd, E, Q, Qt, Kb, Qh)]
            C3 = C.ap.rearrange("p (c t) -> p c t", t=64)
            A3 = A.ap.rearrange("p (c t) -> p c t", t=64)
            B3 = B.ap.rearrange("p (c t) -> p c t", t=64)
            D3 = Dd.ap.rearrange("p (c t) -> p c t", t=64)
            tstate = {"i": 0}

            def talloc():
                p_ = pairs[2 + tstate["i"] % 2]
                tstate["i"] += 1
                return p_

            Qt_b, Kb_b, Qh_b = [RA.get(1024, BF16) for _ in range(3)]
            sm_b = RA.get(128, F32)
            Kp_b = TB(recvb.ap[:, 0:1024].bitcast(BF16))
            Kp_b.buf.r = list(spill)
            P.op("dve", lambda v: v.memset(Kp_b.ap, 0.0), writes=[Kp_b])
            PAR = []
            for (qt_, kb_, qh_, kp_, sm_) in ((Qt, Kb, Qh, Kp, sm), (Qt_b, Kb_b, Qh_b, Kp_b, sm_b)):
                d_ = {"Qth": halves(qt_), "Kbh": halves(kb_), "Qhh": halves(qh_), "Kp": kp_,
                      "Kp4": kp_.ap.rearrange("p (b two d) -> p b two d", two=2, d=128),
                      "Kp3": kp_.ap.rearrange("p (c d) -> p c d", d=128),
                      "Qt": qt_, "Kb": kb_, "Qh": qh_, "sm": sm_}
                for i_, nm_ in enumerate(("dLm", "eLm", "aC", "BcI", "Bx")):
                    d_[nm_] = sm_.ap[:, i_ * 16:(i_ + 1) * 16]
                PAR.append(d_)

            def emit_proj(h_):
                s_ = wload("qf", li * 8 + h_, 4096)
                wv_ = s_.ap.rearrange("p (g k m) -> p g k m", g=2, k=KC)
                mm32(pairs[0], lambda kc, wv_=wv_: wv_[:, 0, kc, :], [s_])
                mm32(pairs[1], lambda kc, wv_=wv_: wv_[:, 1, kc, :], [s_])

            def part1(h):
                pr = PAR[h % 2]
                Qth, Kbh, Qhh = pr["Qth"], pr["Kbh"], pr["Qhh"]
                Kp4_, smb = pr["Kp4"], pr["sm"]
                dLm, eLm, aC, BcI, Bx = pr["dLm"], pr["eLm"], pr["aC"], pr["BcI"], pr["Bx"]
                pq = pairs[0]
                pz = pairs[1]
                lbc = lb_v[:, l, h:h + 1]
                omlc = oml_v[:, l, h:h + 1]
                H2 = (0, 1)
                for tt in H2:
                    P.op("act", lambda a, tt=tt: a.activation(out=Ah[tt].ap, in_=pz.ap[:, tt * 512:(tt + 1) * 512], func=AF.Sigmoid), reads=[pz], writes=[Ah[tt]])
                    yield
                for tt in H2:
                    P.op("act", lambda a, tt=tt: a.activation(out=Bh[tt].ap, in_=pz.ap[:, tt * 512:(tt + 1) * 512], func=AF.Sigmoid, scale=-1.0), reads=[pz], writes=[Bh[tt]])
                    yield
                for tt in H2:
                    P.op("act", lambda a, tt=tt: a.activation(out=Qs[tt].ap, in_=pq.ap[:, tt * 512:(tt + 1) * 512], func=AF.Copy), reads=[pq], writes=[Qs[tt]])
                    yield
                if h + 1 < 8:
                    emit_proj(h + 1)
                    yield
                for tt in H2:
                    P.op("dve", lambda v, tt=tt: v.tensor_scalar(out=Ah[tt].ap, in0=Ah[tt].ap, scalar1=omlc, scalar2=lbc, op0=ALU.mult, op1=ALU.add), reads=[Ah[tt], LB], writes=[Ah[tt]])
                    yield
                for tt in H2:
                    P.op("dve", lambda v, tt=tt: v.tensor_scalar(out=Ah[tt].ap, in0=Ah[tt].ap, scalar1=1e-30, scalar2=None, op0=ALU.max), reads=[Ah[tt]], writes=[Ah[tt]])
                    yield
                for tt in H2:
                    P.op("act", lambda a, tt=tt: a.activation(out=Ah[tt].ap, in_=Ah[tt].ap, func=AF.Ln), reads=[Ah[tt]], writes=[Ah[tt]])
                    yield
                for tt in H2:
                    P.op("dve", lambda v, tt=tt: v.tensor_tensor_scan(out=Ch[tt].ap, data0=rmask[:, tt * 512:(tt + 1) * 512], data1=Ah[tt].ap, initial=0.0, op0=ALU.mult, op1=ALU.add), reads=[Ah[tt], CST], writes=[Ch[tt]])
                    yield
                for tt in H2:
                    P.op("dve", lambda v, tt=tt: v.tensor_tensor(out=v3(Dh[tt]), in0=v3(Ch[tt]), in1=v3(Ch[tt])[:, :, 31:32].broadcast_to([128, 8, 64]), op=ALU.subtract), reads=[Ch[tt]], writes=[Dh[tt]])
                    yield
                for tt in H2:
                    P.op("act", lambda a, tt=tt: a.activation(out=Eh[tt].ap, in_=Dh[tt].ap, func=AF.Exp), reads=[Dh[tt]], writes=[Eh[tt]])
                    yield
                for tt in H2:
                    P.op("dve", lambda v, tt=tt: v.tensor_tensor(out=Qth[tt].ap, in0=Qs[tt].ap, in1=Eh[tt].ap, op=ALU.mult), reads=[Qs[tt], Eh[tt]], writes=[Qth[tt]])
                    yield
                for tt in H2:
                    P.op("act", lambda a, tt=tt: a.activation(out=Eh[tt].ap, in_=Dh[tt].ap, func=AF.Exp, scale=-1.0), reads=[Dh[tt]], writes=[Eh[tt]])
                    yield
                for tt in H2:
                    P.op("dve", lambda v, tt=tt: v.scalar_tensor_tensor(out=Bh[tt].ap, in0=Bh[tt].ap, scalar=omlc, in1=Eh[tt].ap, op0=ALU.mult, op1=ALU.mult), reads=[Bh[tt], Eh[tt], LB], writes=[Bh[tt]])
                    yield
                for tt in H2:
                    P.op("act", lambda a, tt=tt: a.activation(out=Kbh[tt].ap, in_=Bh[tt].ap, func=AF.Copy), reads=[Bh[tt]], writes=[Kbh[tt]])
                    yield
                P.op("dve", lambda v: v.tensor_tensor(out=dLm, in0=C3[:, :, 63], in1=C3[:, :, 31], op=ALU.subtract), reads=Ch, writes=[smb])
                yield
                P.op("act", lambda a: a.activation(out=eLm, in_=dLm, func=AF.Exp), reads=[smb], writes=[smb])
                yield
                P.op("act", lambda a: a.activation(out=aC, in_=C3[:, :, 63], func=AF.Exp), reads=Ch + [smb], writes=[smb])
                yield
                for tt in H2:
                    P.op("dve", lambda v, tt=tt: v.tensor_tensor(out=v3(Dh[tt]), in0=v3(Bh[tt]), in1=eLm[:, tt * 8:(tt + 1) * 8].unsqueeze(2).broadcast_to([128, 8, 64]), op=ALU.mult), reads=[Bh[tt], smb], writes=[Dh[tt]])
                    yield
                ptr = talloc()
                P.group("pe", [
                    (lambda t, tb=tb: t.transpose(out=ptr.ap[:, tb * 128:(tb + 1) * 128], in_=Dd.ap[:, tb * 128:(tb + 1) * 128], identity=ident))
                    for tb in range(8)], reads=Dh + [CST], writes=[ptr])
                yield
                ptr3 = ptr.ap.rearrange("p (b d) -> p b d", d=128)
                P.op("act", lambda a: a.activation(out=Kp4_[0:64, :, 0, :], in_=ptr3[0:64, :, :], func=AF.Copy), reads=[ptr], writes=[pr["Kp"]])
                yield
                P.op("act", lambda a: a.activation(out=Kp4_[64:128, :, 1, :], in_=ptr3[64:128, :, :], func=AF.Copy), reads=[ptr], writes=[pr["Kp"]])
                yield
                for tt in H2:
                    P.op("act", lambda a, tt=tt: a.activation(out=Eh[tt].ap, in_=Ch[tt].ap, func=AF.Exp), reads=[Ch[tt]], writes=[Eh[tt]])
                    yield
                for tt in H2:
                    P.op("dve", lambda v, tt=tt: v.tensor_tensor(out=Qhh[tt].ap, in0=Qs[tt].ap, in1=Eh[tt].ap, op=ALU.mult), reads=[Qs[tt], Eh[tt]], writes=[Qhh[tt]])
                    yield
                P.op("dve", lambda v: v.tensor_tensor_scan(out=BcI, data0=ones16, data1=C3[:, :, 63], initial=0.0, op0=ALU.mult, op1=ALU.add), reads=Ch + [CST, smb], writes=[smb])
                yield
                P.op("dve", lambda v: v.tensor_tensor(out=Bx, in0=BcI, in1=C3[:, :, 63], op=ALU.subtract), reads=Ch + [smb], writes=[smb])
                yield
                for tt in H2:
                    P.op("dve", lambda v, tt=tt: v.tensor_tensor(out=v3(Ah[tt]), in0=v3(Ch[tt]), in1=Bx[:, tt * 8:(tt + 1) * 8].unsqueeze(2).broadcast_to([128, 8, 64]), op=ALU.add), reads=[Ch[tt], smb], writes=[Ah[tt]])
                    yield
                for tt in H2:
                    P.op("act", lambda a, tt=tt: a.activation(out=Eh[tt].ap, in_=Ah[tt].ap, func=AF.Exp), reads=[Ah[tt]], writes=[Eh[tt]])
                    yield
                for tt in H2:
                    P.op("dve", lambda v, tt=tt: v.tensor_tensor(out=Qff3[:, h, tt * 512:(tt + 1) * 512], in0=Qs[tt].ap, in1=Eh[tt].ap, op=ALU.mult), reads=[Qs[tt], Eh[tt]], writes=[Qff])
                    yield

            def tail(h):
                pr = PAR[h % 2]
                Qth, Kbh, Qhh = pr["Qth"], pr["Kbh"], pr["Qhh"]
                Kp3_, smb, aC = pr["Kp3"], pr["sm"], pr["aC"]
                Kb_, Qt_, Qh_ = pr["Kb"], pr["Qt"], pr["Qh"]
                pU = [talloc(), talloc()]
                P.group("pe", [
                    (lambda t, c=c: t.matmul(pU[c // 8].ap[:, (c % 8) * 128:(c % 8 + 1) * 128], lhsT=Kp3_[:, c, :],
                                             rhs=Vv[:, c // 2, h * 128:(h + 1) * 128], start=True, stop=True))
                    for c in range(16)], reads=[pr["Kp"], Vt], writes=pU)
                yield
                P.op("dve", lambda v: v.tensor_copy(out=Sall3[:, 0, :], in_=pU[0].ap[:, 0:128]), reads=[pU[0]], writes=[Sall])
                yield
                for c in range(1, 16):
                    P.op("dve", lambda v, c=c: v.scalar_tensor_tensor(
                        out=Sall3[:, c, :], in0=Sall3[:, c - 1, :], scalar=aC[:, c:c + 1],
                        in1=pU[c // 8].ap[:, (c % 8) * 128:(c % 8 + 1) * 128], op0=ALU.mult, op1=ALU.add),
                        reads=[Sall, smb, pU[c // 8]], writes=[Sall])
                    yield
                P.op("act", lambda a: a.activation(out=Sb.ap, in_=Sall.ap, func=AF.Copy), reads=[Sall], writes=[Sb])
                yield
                P.op("act", lambda a: a.activation(out=sendb.ap[:, h * 128:(h + 1) * 128], in_=Sall3[:, 15, :], func=AF.Copy), reads=[Sall], writes=[sendb])
                yield
                pS = talloc()
                P.group("pe", [
                    (lambda t, c=c: t.matmul(pS.ap[(c % 2) * 64:(c % 2) * 64 + 64, (c // 2) * 64:(c // 2) * 64 + 64],
                                             lhsT=Kb_.ap[:, c * 64:(c + 1) * 64], rhs=Qt_.ap[:, c * 64:(c + 1) * 64],
                                             start=True, stop=True))
                    for c in range(16)], reads=Kbh + Qth, writes=[pS])
                yield
                pS3 = pS.ap[:, 0:512].rearrange("p (b t) -> p b t", t=64)
                P.op("dve", lambda v: v.tensor_tensor(out=Sm4[0:64, :, 0, :], in0=pS3[0:64, :, :], in1=cmask8[0:64, :, :], op=ALU.mult), reads=[pS, CST], writes=[Sm])
                yield
                P.op("dve", lambda v: v.tensor_tensor(out=Sm4[64:128, :, 1, :], in0=pS3[64:128, :, :], in1=cmask8[64:128, :, :], op=ALU.mult), reads=[pS, CST], writes=[Sm])
                yield
                pO = talloc()
                fns = []
                for c in range(16):
                    fns.append(lambda t, c=c: t.matmul(pO.ap[:, c * 64:(c + 1) * 64], lhsT=Vv[:, c // 2, h * 128:(h + 1) * 128],
                                                       rhs=Sm3[:, c, :], start=True, stop=(c == 0)))
                    if c > 0:
                        fns.append(lambda t, c=c: t.matmul(pO.ap[:, c * 64:(c + 1) * 64], lhsT=Sb3[:, c - 1, :],
                                                           rhs=Qh_.ap[:, c * 64:(c + 1) * 64], start=False, stop=True))
                P.group("pe", fns, reads=[Vt, Sm, Sb] + Qhh, writes=[pO])
                yield
                P.op("act", lambda a: a.activation(out=oloc3[:, h, :], in_=pO.ap, func=AF.Copy), reads=[pO], writes=[oloc])
                yield

            def drain(*gens):
                gens = list(gens)
                while gens:
                    for g_ in list(gens):
                        try:
                            next(g_)
                        except StopIteration:
                            gens.remove(g_)

            emit_proj(0)
            drain(part1(0))
            for h in range(8):
                if h + 1 < 8:
                    drain(part1(h + 1), tail(h))
                else:
                    drain(tail(h))
            if MIXCUT == 1:
                reload_x()
                return
            P.barrier()
            RB.reset()
            for a_ in RY:
                a_.reset()
            yconv = RY[0].get(8192, BF16)
            yh = RY[1]
            ymem_a = RY[2]
            yc3 = yconv.ap.rearrange("p (j t) -> p j t", j=8)
            xs = RB.get(1024, F32)
            ub = RB.get(1040, F32)
            acc = RB.get(1024, F32)
            P.op("dve", lambda v: v.memset(ub.ap[:, 0:16], 0.0), writes=[ub])
            for j in range(8):
                s1 = wload("cxc", li * 8 + j, 4096)
                wv1 = s1.ap.rearrange("p (g k m) -> p g k m", g=2, k=KC)
                s2 = wload("cb", li * 8 + j, 2048)
                wv2 = s2.ap[:, 0:2048].rearrange("p (k m) -> p k m", k=KC)
                pX = palloc()
                mm32(pX, lambda kc, wv1=wv1: wv1[:, 0, kc, :], [s1])
                pC = palloc()
                mm32(pC, lambda kc, wv1=wv1: wv1[:, 1, kc, :], [s1])
                pB = palloc()
                mm32(pB, lambda kc, wv2=wv2: wv2[:, kc, :], [s2])
                w0 = pcol("cw", l * 24 + 0 * 8 + j)
                w1 = pcol("cw", l * 24 + 1 * 8 + j)
                w2 = pcol("cw", l * 24 + 2 * 8 + j)
                P.op("act", lambda a, pX=pX: a.activation(out=xs.ap, in_=pX.ap, func=AF.Copy), reads=[pX], writes=[xs])
                P.op("dve", lambda v, pC=pC: v.tensor_tensor(out=ub.ap[:, 16:1040], in0=pC.ap, in1=xs.ap, op=ALU.mult), reads=[pC, xs], writes=[ub])
                if CONVDBG >= 2:
                    P.op("dve", lambda v, j=j, pB=pB: v.tensor_tensor(out=yc3[:, j, :], in0=pB.ap, in1=ub.ap[:, 16:1040], op=ALU.mult), reads=[pB, ub], writes=[yconv])
                    continue
                P.op("act", lambda a, w1=w1: a.activation(out=xs.ap, in_=ub.ap[:, 15:1039], func=AF.Identity, scale=w1), reads=[ub, par], writes=[xs])
                P.op("dve", lambda v, w2=w2: v.scalar_tensor_tensor(out=acc.ap, in0=ub.ap[:, 16:1040], scalar=w2, in1=xs.ap, op0=ALU.mult, op1=ALU.add), reads=[ub, par, xs], writes=[acc])
                P.op("dve", lambda v, w0=w0: v.scalar_tensor_tensor(out=acc.ap, in0=ub.ap[:, 14:1038], scalar=w0, in1=acc.ap, op0=ALU.mult, op1=ALU.add), reads=[ub, par, acc], writes=[acc])
                P.op("dve", lambda v, j=j, pB=pB: v.tensor_tensor(out=yc3[:, j, :], in0=pB.ap, in1=acc.ap, op=ALU.mult), reads=[pB, acc], writes=[yconv])
                if CONVDBG >= 1:
                    continue
                P.op("dve", lambda v, j=j: v.tensor_copy(out=sendb.ap[:, 1024 + 16 * j:1040 + 16 * j], in_=ub.ap[:, 1024:1040]), reads=[ub], writes=[sendb])
                P.op("dve", lambda v, j=j: v.tensor_copy(out=cpre[:, 16 * j:16 * j + 16], in_=acc.ap[:, 0:16]), reads=[acc], writes=[xsm])
                P.op("dve", lambda v, j=j, pB=pB: v.tensor_copy(out=cbs[:, 16 * j:16 * j + 16], in_=pB.ap[:, 0:16]), reads=[pB], writes=[xsm])
            if MIXCUT == 2:
                reload_x()
                return
            t_s = P.dma("pool", lambda g: g.dma_start(out=ccs_d[:, :], in_=sendb.ap), "cc_s", reads=[sendb])
            t_c = P.custom("pool", lambda g: g.collective_compute("AllGather", ALU.bypass, replica_groups=rg_pairs,
                                                                  ins=[ccs_d[:, :]], outs=[ccd_d[:, :]]), "cc_c", 1, extra=[t_s])
            P.dma("pool", lambda g: g.dma_start(out=recvb.ap, in_=ccd_d[0:128, :]), "cc_r", writes=[recvb], extra=[t_c])
            if MIXCUT == 3:
                P.wait("dve", [P.dcnt and ("cc_r", P.dcnt["cc_r"])])
                reload_x()
                return
            P.barrier()
            RB.reset()
            RY[1].reset()
            memraw = RB.get(4096, F32)
            mr3 = memraw.ap.rearrange("p (c m) -> p c m", c=KC)
            memn = RB.get(4096, BF16)
            mn3 = memn.ap.rearrange("p (c m) -> p c m", c=KC)
            kT = RY[1].get(2048, BF16)
            kT3 = kT.ap.rearrange("p (j m) -> p j m", j=8)
            vM = RY[1].get(2048, BF16)
            vM3 = vM.ap.rearrange("p (b n) -> p b n", b=2)
            bt3 = [(e, P.E[e].cnt) for e in ("pe", "act", "dve")]
            for q in range(4):
                P.dma("sp", lambda e, q=q: e.dma_start(out=mr3[:, q * 4:(q + 1) * 4, :],
                                                       in_=mem_d[q * 512:(q + 1) * 512, :].rearrange("(c p) m -> p c m", p=128)),
                      "ld_mem%d" % q, writes=[memraw], extra=bt3)
            rstd = rms_stats(lambda c: mr3[:, c, :], memraw, KC, 256, 1.0 / D, RY[1])
            for c in range(KC):
                P.op("dve", lambda v, c=c: v.scalar_tensor_tensor(out=mn3[:, c, :], in0=mr3[:, c, :], scalar=pcol("nmem", l * 16 + c),
                                                                  in1=rstd.ap, op0=ALU.mult, op1=ALU.mult), reads=[memraw, rstd, par], writes=[memn])
            for jp in range(4):
                s = wload("kk", li * 4 + jp, 4096)
                wv = s.ap.rearrange("p (g k m) -> p g k m", g=2, k=KC)
                pair = palloc()
                P.group("pe", [
                    (lambda t, j2=j2, kc=kc, pair=pair, wv=wv: t.matmul(pair.ap[:, j2 * 256:(j2 + 1) * 256], lhsT=wv[:, j2, kc, :],
                                                                       rhs=mn3[:, kc, :], start=(kc == 0), stop=(kc == KC - 1)))
                    for j2 in range(2) for kc in range(KC)], reads=[s, memn], writes=[pair])
                P.op("act", lambda a, jp=jp, pair=pair: a.activation(out=kT3[:, jp * 2:(jp + 1) * 2, :],
                                                                     in_=pair.ap[:, 0:512].rearrange("p (j m) -> p j m", j=2), func=AF.Copy),
                     reads=[pair], writes=[kT])
            for vq in range(4):
                s = wload("kv", li * 4 + vq, 4096)
                wv = s.ap.rearrange("p (k n) -> p k n", k=KC)
                pair = palloc()
                P.group("pe", [
                    (lambda t, mb=mb, kc=kc, pair=pair, wv=wv: t.matmul(pair.ap[:, mb * 256:(mb + 1) * 256],
                                                                       lhsT=mn3[:, kc, mb * 128:(mb + 1) * 128],
                                                                       rhs=wv[:, kc, :], start=(kc == 0), stop=(kc == KC - 1)))
                    for mb in range(2) for kc in range(KC)], reads=[s, memn], writes=[pair])
                P.op("act", lambda a, vq=vq, pair=pair: a.activation(out=vM3[:, :, vq * 256:(vq + 1) * 256],
                                                                     in_=pair.ap[:, 0:512].rearrange("p (b n) -> p b n", b=2), func=AF.Copy),
                     reads=[pair], writes=[vM])
            P.barrier()
            RB.reset()
            ymem = RY[2].get(8192, BF16)
            ym3 = ymem.ap.rearrange("p (j t) -> p j t", j=8)
            qa = RB.get(2048, BF16)
            qa3 = qa.ap.rearrange("p (d t) -> p d t", d=2)
            Eb = RB.get(2048, BF16)
            Eb3 = Eb.ap.rearrange("p (b t) -> p b t", b=2)
            rden = RB.get(1024, F32)
            for a_ in range(4):
                s = wload("aq", li * 4 + a_, 4096)
                wv = s.ap.rearrange("p (g k m) -> p g k m", g=2, k=KC)
                for d2 in range(2):
                    pair = palloc()
                    mm32(pair, lambda kc, wv=wv, d2=d2: wv[:, d2, kc, :], [s])
                    P.op("act", lambda a, d2=d2, pair=pair: a.activation(out=qa3[:, d2, :], in_=pair.ap, func=AF.Copy), reads=[pair], writes=[qa])
                for mb in range(2):
                    pair = palloc()
                    P.group("pe", [
                        (lambda t, tt=tt, d2=d2, mb=mb, a_=a_, pair=pair: t.matmul(
                            pair.ap[:, tt * 512:(tt + 1) * 512], lhsT=kT3[:, 2 * a_ + d2, mb * 128:(mb + 1) * 128],
                            rhs=qa3[:, d2, tt * 512:(tt + 1) * 512], start=(d2 == 0), stop=(d2 == 1)))
                        for tt in range(2) for d2 in range(2)], reads=[kT, qa], writes=[pair])
                    P.op("act", lambda a, mb=mb, pair=pair: a.activation(out=Eb3[:, mb, :], in_=pair.ap, func=AF.Exp, scale=1.0 / 16.0),
                         reads=[pair], writes=[Eb])
                pair = palloc()
                P.group("pe", [
                    (lambda t, tt=tt, mb=mb, pair=pair: t.matmul(pair.ap[:, tt * 512:(tt + 1) * 512], lhsT=ones_b,
                                                                rhs=Eb3[:, mb, tt * 512:(tt + 1) * 512], start=(mb == 0), stop=(mb == 1)))
                    for tt in range(2) for mb in range(2)], reads=[Eb, CST], writes=[pair])
                P.op("dve", lambda v, pair=pair: v.reciprocal(out=rden.ap, in_=pair.ap), reads=[pair], writes=[rden])
                for dv2 in range(2):
                    pair = palloc()
                    P.group("pe", [
                        (lambda t, tt=tt, mb=mb, a_=a_, dv2=dv2, pair=pair: t.matmul(
                            pair.ap[:, tt * 512:(tt + 1) * 512], lhsT=vM3[:, mb, a_ * 256 + dv2 * 128:a_ * 256 + dv2 * 128 + 128],
                            rhs=Eb3[:, mb, tt * 512:(tt + 1) * 512], start=(mb == 0), stop=(mb == 1)))
                        for tt in range(2) for mb in range(2)], reads=[vM, Eb], writes=[pair])
                    P.op("dve", lambda v, a_=a_, dv2=dv2, pair=pair: v.tensor_tensor(out=ym3[:, 2 * a_ + dv2, :], in0=pair.ap, in1=rden.ap, op=ALU.mult),
                         reads=[pair, rden], writes=[ymem])
            if MIXCUT == 4:
                P.wait("dve", [("cc_r", P.dcnt["cc_r"])])
                reload_x()
                return
            P.barrier()
            RB.reset()
            RY[1].reset()
            yhb = RY[1].get(8192, BF16)
            yh3 = yhb.ap.rearrange("p (h t) -> p h t", h=8)
            FS = [{"sq": RB.get(1024, BF16), "rs": RB.get(1024, F32), "sg": RB.get(1024, F32)} for _ in range(2)]
            ol = [TB(oloc3[:, h_, :]) for h_ in range(8)]
            flag = pcol("flag", 0)
            P.op("dve", lambda v: v.tensor_scalar(out=Sin.ap, in0=recvb.ap[:, 0:1024], scalar1=flag, scalar2=None, op0=ALU.mult), reads=[recvb, par], writes=[Sin])
            P.op("dve", lambda v: v.tensor_scalar(out=uprev, in0=recvb.ap[:, 1024:1152], scalar1=flag, scalar2=None, op0=ALU.mult), reads=[recvb, par], writes=[xsm])

            def fin(h, fs):
                sq, rs, sg = fs["sq"], fs["rs"], fs["sg"]
                pair = palloc()
                P.group("pe", [
                    (lambda t, tt=tt: t.matmul(pair.ap[:, tt * 512:(tt + 1) * 512], lhsT=Sin3[:, h, :],
                                               rhs=Qff3[:, h, tt * 512:(tt + 1) * 512], start=True, stop=True))
                    for tt in range(2)], reads=[Sin, Qff], writes=[pair])
                yield
                P.op("dve", lambda v: v.tensor_tensor(out=ol[h].ap, in0=ol[h].ap, in1=pair.ap, op=ALU.add), reads=[pair, ol[h]], writes=[ol[h]])
                yield
                P.op("act", lambda a: a.activation(out=sq.ap, in_=ol[h].ap, func=AF.Square), reads=[ol[h]], writes=[sq])
                yield
                pair2 = palloc()
                P.group("pe", [
                    (lambda t, tt=tt: t.matmul(pair2.ap[:, tt * 512:(tt + 1) * 512], lhsT=ones_b, rhs=sq.ap[:, tt * 512:(tt + 1) * 512],
                                               start=True, stop=True)) for tt in range(2)], reads=[sq, CST], writes=[pair2])
                yield
                P.op("act", lambda a: a.activation(out=rs.ap, in_=pair2.ap, func=AF.Ln, bias=epsb, scale=1.0 / 128.0), reads=[pair2, CST], writes=[rs])
                yield
                P.op("act", lambda a: a.activation(out=rs.ap, in_=rs.ap, func=AF.Exp, scale=-0.5), reads=[rs], writes=[rs])
                yield
                s_ = wload("hg", li * 8 + h, 2048)
                wv_ = s_.ap[:, 0:2048].rearrange("p (k m) -> p k m", k=KC)
                pg = palloc()
                mm32(pg, lambda kc: wv_[:, kc, :], [s_])
                yield
                P.op("act", lambda a: a.activation(out=sg.ap, in_=pg.ap, func=AF.Silu), reads=[pg], writes=[sg])
                yield
                P.op("dve", lambda v: v.scalar_tensor_tensor(out=ol[h].ap, in0=ol[h].ap, scalar=pcol("hn", l), in1=rs.ap, op0=ALU.mult, op1=ALU.mult),
                     reads=[ol[h], rs, par], writes=[ol[h]])
                yield
                P.op("dve", lambda v: v.tensor_tensor(out=yh3[:, h, :], in0=ol[h].ap, in1=sg.ap, op=ALU.mult), reads=[ol[h], sg], writes=[yhb])
                yield

            for hp_ in range(4):
                drain(fin(2 * hp_, FS[0]), fin(2 * hp_ + 1, FS[1]))
            cw0 = par_t[:, PO["cw"] + l * 24:PO["cw"] + l * 24 + 8]
            cw1 = par_t[:, PO["cw"] + l * 24 + 8:PO["cw"] + l * 24 + 16]
            up3 = uprev.rearrange("p (j k) -> p j k", k=16)
            cp3 = cpre.rearrange("p (j k) -> p j k", k=16)
            cb3 = cbs.rearrange("p (j k) -> p j k", k=16)
            t13 = t1s.rearrange("p (j k) -> p j k", k=2)
            t23 = t2s.rearrange("p (j k) -> p j k", k=2)
            yf3 = yfix.rearrange("p (j k) -> p j k", k=2)
            w0b = cw0.unsqueeze(2).broadcast_to([128, 8, 2])
            w1b = cw1.unsqueeze(2).broadcast_to([128, 8, 2])
            P.op("dve", lambda v: v.tensor_tensor(out=t13, in0=up3[:, :, 14:16], in1=w0b, op=ALU.mult), reads=[xsm, par], writes=[xsm])
            P.op("dve", lambda v: v.tensor_tensor(out=cp3[:, :, 0:2], in0=cp3[:, :, 0:2], in1=t13, op=ALU.add), reads=[xsm], writes=[xsm])
            P.op("dve", lambda v: v.tensor_tensor(out=t23, in0=up3[:, :, 15:16].broadcast_to([128, 8, 2]), in1=w1b, op=ALU.mult), reads=[xsm, par], writes=[xsm])
            P.op("dve", lambda v: v.tensor_tensor(out=t23, in0=t23, in1=mask10, op=ALU.mult), reads=[xsm, CST], writes=[xsm])
            P.op("dve", lambda v: v.tensor_tensor(out=cp3[:, :, 0:2], in0=cp3[:, :, 0:2], in1=t23, op=ALU.add), reads=[xsm], writes=[xsm])
            P.op("dve", lambda v: v.tensor_tensor(out=yf3, in0=cb3[:, :, 0:2], in1=cp3[:, :, 0:2], op=ALU.mult), reads=[xsm], writes=[xsm])
            P.op("dve", lambda v: v.tensor_copy(out=yc3[:, :, 0:2], in_=yf3), reads=[xsm], writes=[yconv])
            reload_x()
            if MIXCUT == 5:
                return
            RB.reset()
            sgm = RB.get(1024, F32)
            macc = RB.get(1024, F32)
            tmp = RB.get(1024, F32)
            mg = RB.get(4096, BF16)
            mg3 = mg.ap.rearrange("p (k t) -> p k t", k=4)
            Ys = [yc3, yh3, ym3]
            Yb = [yconv, yhb, ymem]
            for grp in range(4):
                for d4 in range(4):
                    dc = grp * 4 + d4
                    for n in range(3):
                        s = wload("mg", (li * 16 + dc) * 3 + n, 3072)
                        gw = s.ap[:, 0:2048].rearrange("p (k m) -> p k m", k=KC)
                        bw = s.ap[:, 2048:3072].rearrange("p (k m) -> p k m", k=8)
                        pG = palloc()
                        mm32(pG, lambda kc, gw=gw: gw[:, kc, :], [s])
                        pP = palloc()
                        P.group("pe", [
                            (lambda t, k8=k8, tt=tt, n=n, pP=pP, bw=bw: t.matmul(pP.ap[:, tt * 512:(tt + 1) * 512], lhsT=bw[:, k8, :],
                                                                                rhs=Ys[n][:, k8, tt * 512:(tt + 1) * 512],
                                                                                start=(k8 == 0), stop=(k8 == 7)))
                            for k8 in range(8) for tt in range(2)], reads=[s, Yb[n]], writes=[pP])
                        P.op("act", lambda a, pG=pG: a.activation(out=sgm.ap, in_=pG.ap, func=AF.Sigmoid), reads=[pG], writes=[sgm])
                        if n == 0:
                            P.op("dve", lambda v, pP=pP: v.tensor_tensor(out=macc.ap, in0=sgm.ap, in1=pP.ap, op=ALU.mult), reads=[sgm, pP], writes=[macc])
                        else:
                            P.op("dve", lambda v, pP=pP: v.tensor_tensor(out=tmp.ap, in0=sgm.ap, in1=pP.ap, op=ALU.mult), reads=[sgm, pP], writes=[tmp])
                            if n == 1:
                                P.op("dve", lambda v: v.tensor_tensor(out=macc.ap, in0=macc.ap, in1=tmp.ap, op=ALU.add), reads=[macc, tmp], writes=[macc])
                            else:
                                P.op("dve", lambda v, d4=d4: v.tensor_tensor(out=mg3[:, d4, :], in0=macc.ap, in1=tmp.ap, op=ALU.add), reads=[macc, tmp], writes=[mg])
                for half in range(2):
                    s = wload("wo", (li * 4 + grp) * 2 + half, 4096)
                    wv = s.ap.rearrange("p (d k m) -> p d k m", d=8, k=4)
                    for d8 in range(8):
                        dcq = half * 8 + d8
                        pair = palloc()
                        P.group("pe", [
                            (lambda t, k4=k4, tt=tt, d8=d8, pair=pair, wv=wv: t.matmul(pair.ap[:, tt * 512:(tt + 1) * 512], lhsT=wv[:, d8, k4, :],
                                                                                      rhs=mg3[:, k4, tt * 512:(tt + 1) * 512],
                                                                                      start=(k4 == 0), stop=(k4 == 3)))
                            for k4 in range(4) for tt in range(2)], reads=[s, mg], writes=[pair])
                        P.op("dve", lambda v, dcq=dcq, pair=pair: v.tensor_tensor(out=xT.ap[:, dcq, :], in0=pair.ap, in1=xT.ap[:, dcq, :], op=ALU.add),
                             reads=[pair, xT], writes=[xT])

        for (kind, l) in stages:
            if kind == "ffn1":
                ffn(l, 1)
            elif kind == "ffn2":
                ffn(l, 2)
            else:
                mixer(l)

        RA.reset()
        P.barrier()
        outb = []
        if final_norm:
            rstd = rms_stats(lambda c: xT.ap[:, c, :], xT, KC, T, 1.0 / D, RA)
            ot = [RA.get(T, F32) for _ in range(2)]
            for c in range(KC):
                o = ot[c % 2]
                P.op("dve", lambda v, c=c, o=o: v.scalar_tensor_tensor(
                    out=o.ap, in0=xT.ap[:, c, :], scalar=pcol("nf", c), in1=rstd.ap,
                    op0=ALU.mult, op1=ALU.mult), reads=[xT, rstd, par], writes=[o])
                tk = P.dma("sp", lambda e, c=c, o=o: e.dma_start(out=y_d[c * 128:(c + 1) * 128, :], in_=o.ap),
                           "st%d" % (c % 2), reads=[o])
                outb.append(tk)
        else:
            for q in range(4):
                tk = P.dma("sp", lambda e, q=q: e.dma_start(
                    out=y_d[q * 512:(q + 1) * 512, :].rearrange("(c p) t -> p c t", p=128),
                    in_=xT.ap[:, q * 4:(q + 1) * 4, :]), "st%d" % q, reads=[xT])
                outb.append(tk)
        P.wait("sp", outb)

        semkeys = ["pe", "act", "dve", "pool", "sp"] + sorted(P.dcnt.keys())
        sems = {}
        for k in semkeys:
            sems[k] = es.enter_context(nc.semaphore("s_" + k))
        with nc.Block() as block:
            def replay(en, h):
                for o in P.E[en].ops:
                    if o[0] == "wait":
                        h.wait_ge(sems[o[1]], o[2])
                    elif o[0] == "op":
                        ins = o[1](h)
                        if o[2]:
                            ins.then_inc(sems[en], 1)
                    elif o[0] == "custom":
                        ins = o[1](h)
                        ins.then_inc(sems[o[2]], o[3])
                    else:
                        ins = o[1](h)
                        ins.then_inc(sems[o[2]], 16)

            @block.tensor
            def _(t):
                replay("pe", t)

            @block.scalar
            def _(a):
                replay("act", a)

            @block.vector
            def _(v):
                replay("dve", v)

            @block.gpsimd
            def _(g):
                replay("pool", g)

            @block.sync
            def _(s):
                replay("sp", s)
    return nc, sorted(W.keys())


def _lhsT(Wl, c0, ncols=128):
    return Wl[:, c0:c0 + ncols].reshape(KC, 128, ncols).transpose(1, 0, 2)


def prep_weights(inp, layers, used):
    out = {}
    NLL = len(layers)
    ls = list(layers)
    for w in (1, 2):
        if ("gu%d" % w) not in used:
            continue
        Wg = inp["ffn%d_w_gate" % w][ls].reshape(NLL, KC, 128, NF, 128)
        Wu = inp["ffn%d_w_up" % w][ls].reshape(NLL, KC, 128, NF, 128)
        gu = np.empty((NLL, NF, 128, 2, KC, 128), np.float32)
        gu[:, :, :, 0] = Wg.transpose(0, 3, 2, 1, 4)
        gu[:, :, :, 1] = Wu.transpose(0, 3, 2, 1, 4)
        out["w_gu%d" % w] = gu.reshape(NLL * NF, 128, 4096)
        Wd = inp["ffn%d_w_down" % w][ls].reshape(NLL, NG, GJ, 128, 8, 2, 128)
        out["w_d%d" % w] = np.ascontiguousarray(Wd.transpose(0, 1, 4, 3, 5, 2, 6)).reshape(NLL * NG * 8, 128, 2816)
    fam = {k: [] for k in ("qf", "vi", "hg", "cxc", "cb", "aq", "mg", "wo", "kk", "kv")}
    for l in (layers if "qf" in used else []):
        Win = inp["w_in"][l]
        Wkv = inp["w_mem_kv"][l]
        Wb = inp["w_branch"][l]
        Wo = inp["w_o"][l]
        for h in range(8):
            fam["qf"].append(np.stack([_lhsT(Win, 3072 + h * 128), _lhsT(Win, 4096 + h * 128)], 1).reshape(128, 4096))
            fam["hg"].append(_lhsT(Win, 6144 + h * 128).reshape(128, 2048))
            fam["cxc"].append(np.stack([_lhsT(Win, h * 128), _lhsT(Win, 2048 + h * 128)], 1).reshape(128, 4096))
            fam["cb"].append(_lhsT(Win, 1024 + h * 128).reshape(128, 2048))
        for q in range(4):
            fam["vi"].append(_lhsT(Win, 5120 + q * 256, 256).reshape(128, 4096))
            fam["aq"].append(np.stack([_lhsT(Win, 7168 + q * 256), _lhsT(Win, 7168 + q * 256 + 128)], 1).reshape(128, 4096))
            fam["kk"].append(np.stack([_lhsT(Wkv, 2 * q * 128), _lhsT(Wkv, (2 * q + 1) * 128)], 1).reshape(128, 4096))
            fam["kv"].append(_lhsT(Wkv, 1024 + q * 256, 256).reshape(128, 4096))
        for dc in range(16):
            for n in range(3):
                g_ = _lhsT(Win, 8192 + n * 2048 + dc * 128).reshape(128, 2048)
                b_ = Wb[n][:, dc * 128:(dc + 1) * 128].reshape(8, 128, 128).transpose(1, 0, 2).reshape(128, 1024)
                fam["mg"].append(np.concatenate([g_, b_], 1))
        wo = Wo.reshape(4, 4, 128, 2, 8, 128).transpose(0, 3, 2, 4, 1, 5).reshape(8, 128, 4096)
        fam["wo"].extend(list(wo))
    for k, v in fam.items():
        if not v:
            continue
        out["w_" + k] = np.ascontiguousarray(np.stack(v, 0), dtype=np.float32)
    return out


def prep_small(inp, core):
    par = np.zeros((128, NPAR), np.float32)

    def put(name, arr):
        par[:, PO[name]:PO[name] + arr.shape[1]] = arr

    for nm, key in (("n1", "norm_ffn1"), ("nm", "norm_mix"), ("n2", "norm_ffn2"), ("nmem", "mem_norm")):
        put(nm, inp[key].reshape(L, KC, 128).transpose(2, 0, 1).reshape(128, L * KC))
    put("nf", inp["final_norm"].reshape(KC, 128).T)
    put("cw", inp["conv_w"].reshape(L, 3, 8, 128).transpose(3, 0, 1, 2).reshape(128, L * 24))
    put("lbl", inp["hgrn_lb_logits"].reshape(L, 8, 128).transpose(2, 0, 1).reshape(128, L * 8))
    put("hn", inp["hgrn_norm"].T)
    par[:, PO["flag"]] = float(core % 2)
    return par


def make_consts():
    c = np.zeros((128, 2048), np.float32)
    c[:, 0:128] = np.eye(128, dtype=np.float32)
    rm = np.ones((128, 1024), np.float32)
    rm[:, 0::64] = 0.0
    c[:, 128:1152] = rm
    s = np.arange(128)[:, None] % 64
    t = np.arange(64)[None, :]
    c[:, 1152:1664] = np.tile((s <= t).astype(np.float32), (1, 8))
    c[:, 1664:1680] = 1.0
    c[:, 1680:1696] = np.tile(np.array([1.0, 0.0], np.float32), 8)
    return c


STAGES_FULL = [(k, l) for l in range(L) for k in ("ffn1", "mix", "ffn2")]
_cache = {}


def run(inp, stages, final_norm=True, trace=False):
    key = (tuple(stages), final_norm)
    if key not in _cache:
        _cache[key] = build(stages, final_norm)
    nc, used = _cache[key]
    x = np.asarray(inp["x"], np.float32)
    mem = np.asarray(inp["mem"], np.float32)
    import time as _t
    _t0 = _t.time()
    layers = sorted({l for _, l in stages})
    wts = prep_weights(inp, layers, used)
    print("[kernel] host prep %.1fs" % (_t.time() - _t0), flush=True)
    cst = make_consts()
    in_maps = []
    for c in range(8):
        b, h = c // 2, c % 2
        m = {"xT": np.ascontiguousarray(x[b, h * T:(h + 1) * T, :].T),
             "memT": np.ascontiguousarray(mem[b].T),
             "par": prep_small(inp, c), "cst": cst}
        m.update({("w_" + k): wts["w_" + k] for k in used})
        in_maps.append(m)
    res = run_bass_kernel_spmd(nc, in_maps, core_ids=list(range(8)), trace=trace)
    out = np.empty((4, 2048, D), np.float32)
    for c in range(8):
        b, h = c // 2, c % 2
        out[b, h * T:(h + 1) * T, :] = res.results[c]["yT"].T
    return out, res


def kernel(**inputs):
    inp = {k: np.asarray(v) for k, v in inputs.items()}
    out, _ = run(inp, STAGES_FULL, True)
    return out
```

```python
import numpy as np
import ml_dtypes
import concourse.bass as bass
import concourse.mybir as mybir
from concourse.bass_utils import run_bass_kernel_spmd

F32 = mybir.dt.float32
BF16 = mybir.dt.bfloat16
AF = mybir.ActivationFunctionType
ALU = mybir.AluOpType

L = 4
D = 2048
T = 1024
KC = 16
FF = 5632
NF = 44
GJ = 11
NG = 4
EPS = 1e-6
MIXCUT = 99
CONVDBG = 0
NSLOT = 3
SLOT = 4096

PO = {}
_o = 0
for _n, _w in [("n1", L * 16), ("nm", L * 16), ("n2", L * 16), ("nmem", L * 16), ("nf", 16),
               ("cw", L * 3 * 8), ("lbl", L * 8), ("hn", L), ("flag", 1)]:
    PO[_n] = _o
    _o += _w
NPAR = _o


class Buf:
    __slots__ = ("w", "r")

    def __init__(self):
        self.w = None
        self.r = []


class TB:
    def __init__(self, ap):
        self.buf = Buf()
        self.ap = ap


class Eng:
    def __init__(self, name):
        self.name = name
        self.ops = []
        self.cnt = 0
        self.seen = {}


class Prog:
    def __init__(self):
        self.E = {n: Eng(n) for n in ["pe", "act", "dve", "pool", "sp"]}
        self.dcnt = {}

    def _deps(self, eng, reads, writes, extra):
        toks = list(extra)
        for b in reads:
            if b.w is not None:
                toks.append(b.w)
        for b in writes:
            if b.w is not None:
                toks.append(b.w)
            toks.extend(b.r)
        for (k, v) in toks:
            if eng.seen.get(k, 0) >= v:
                continue
            eng.seen[k] = v
            eng.ops.append(("wait", k, v))

    def _commit(self, tok, reads, writes):
        for b in reads:
            b.r.append(tok)
        for b in writes:
            b.w = tok
            b.r = []

    def op(self, en, fn, reads=(), writes=(), extra=()):
        return self.group(en, [fn], reads, writes, extra)

    def group(self, en, fns, reads=(), writes=(), extra=()):
        eng = self.E[en]
        reads = [b.buf if isinstance(b, TB) else b for b in reads]
        writes = [b.buf if isinstance(b, TB) else b for b in writes]
        self._deps(eng, reads, writes, extra)
        for fn in fns[:-1]:
            eng.ops.append(("op", fn, False))
        eng.cnt += 1
        tok = (en, eng.cnt)
        eng.ops.append(("op", fns[-1], True))
        self._commit(tok, reads, writes)
        return tok

    def dma(self, en, fn, semkey, reads=(), writes=(), extra=()):
        eng = self.E[en]
        reads = [b.buf if isinstance(b, TB) else b for b in reads]
        writes = [b.buf if isinstance(b, TB) else b for b in writes]
        self._deps(eng, reads, writes, extra)
        self.dcnt[semkey] = self.dcnt.get(semkey, 0) + 16
        tok = (semkey, self.dcnt[semkey])
        eng.ops.append(("dma", fn, semkey))
        self._commit(tok, reads, writes)
        return tok

    def custom(self, en, fn, semkey, inc, reads=(), writes=(), extra=()):
        eng = self.E[en]
        reads = [b.buf if isinstance(b, TB) else b for b in reads]
        writes = [b.buf if isinstance(b, TB) else b for b in writes]
        self._deps(eng, reads, writes, extra)
        self.dcnt[semkey] = self.dcnt.get(semkey, 0) + inc
        tok = (semkey, self.dcnt[semkey])
        eng.ops.append(("custom", fn, semkey, inc))
        self._commit(tok, reads, writes)
        return tok

    def wait(self, en, toks):
        self._deps(self.E[en], [], [], [t for t in toks if t is not None])

    def barrier(self, engs=("pe", "act", "dve"), extra=()):
        toks = [(e, self.E[e].cnt) for e in engs if self.E[e].cnt > 0] + [t for t in extra if t is not None]
        for e in engs:
            if e == "pe":
                continue
            self._deps(self.E[e], [], [], toks)


def build(stages, final_norm=True, ncores=8):
    nc = bass.Bass("TRN2", target_bir_lowering=False)
    P = Prog()
    layers = sorted({l for _, l in stages})
    NL = len(layers)
    LI = {l: i for i, l in enumerate(layers)}

    def din(name, shape, dt=F32):
        return nc.dram_tensor(name, list(shape), dt, kind="ExternalInput").ap()

    x_d = din("xT", [D, T])
    mem_d = din("memT", [D, 256])
    par_d = din("par", [128, NPAR])
    cst_d = din("cst", [128, 2048])
    y_d = nc.dram_tensor("yT", [D, T], F32, kind="ExternalOutput").ap()
    xsp_d = nc.dram_tensor("xspill", [D, T], F32, kind="Internal").ap()
    ccs_d = nc.dram_tensor("cc_src", [128, 1152], F32, kind="Internal").ap()
    ccd_d = nc.dram_tensor("cc_dst", [256, 1152], F32, kind="Internal").ap()
    WSH = {"gu1": [NL * NF, 128, 4096], "gu2": [NL * NF, 128, 4096], "d1": [NL * NG * 8, 128, 2816],
           "d2": [NL * NG * 8, 128, 2816], "qf": [NL * 8, 128, 4096], "vi": [NL * 4, 128, 4096],
           "hg": [NL * 8, 128, 2048], "cxc": [NL * 8, 128, 4096], "cb": [NL * 8, 128, 2048],
           "aq": [NL * 4, 128, 4096], "mg": [NL * 48, 128, 3072], "wo": [NL * 8, 128, 4096],
           "kk": [NL * 4, 128, 4096], "kv": [NL * 4, 128, 4096]}
    W = {}

    def getW(fam):
        if fam not in W:
            W[fam] = din("w_" + fam, WSH[fam])
        return W[fam]

    import contextlib
    es = contextlib.ExitStack()
    with es:
        def sb(name, shape, dt):
            return es.enter_context(nc.sbuf_tensor(name, list(shape), dt))

        hT_t = sb("hT", [128, KC * T], BF16)
        slots_t = sb("slots", [128, NSLOT * SLOT], BF16)
        par_t = sb("par_s", [128, NPAR], F32)
        cstf_t = sb("cstf", [128, 2048], F32)
        cstb_t = sb("cstb", [128, 1152], BF16)
        lb_t = sb("lb", [128, 2 * L * 8 + 16], F32)
        X_t = sb("X", [128, 16384], F32)
        R_t = sb("R", [128, 18432], F32)
        ps_t = es.enter_context(nc.psum_tensor("ps", [128, 4096], F32))

        hT = TB(hT_t[:, :].rearrange("p (c t) -> p c t", c=KC))
        xT = TB(X_t[:, :].rearrange("p (c t) -> p c t", c=KC))
        par = TB(par_t[:, :])
        cstf = cstf_t[:, :]
        ident = cstf[:, 0:128]
        rmask = cstf[:, 128:1152]
        cmask = cstf[:, 1152:1216]
        ones_b = cstb_t[:, 0:128]
        CST = Buf()
        pairs = [TB(ps_t[:, k * 1024:(k + 1) * 1024]) for k in range(4)]
        pstate = {"i": 0}

        reserved = set()
        pend = {}

        def palloc():
            while True:
                k_ = pstate["i"] % 4
                pstate["i"] += 1
                if k_ not in reserved:
                    return pairs[k_]

        slot_bufs = [TB(slots_t[:, k * SLOT:(k + 1) * SLOT]) for k in range(NSLOT)]
        sstate = {"i": 0}

        def wload(fam, idx, n):
            s = slot_bufs[sstate["i"] % NSLOT]
            key = "ws%d" % (sstate["i"] % NSLOT)
            sstate["i"] += 1
            src = getW(fam)[idx]
            dst = s.ap[:, 0:n]
            P.dma("pool", lambda g, dst=dst, src=src: g.dma_start(out=dst, in_=src), key, reads=[], writes=[s])
            return s

        def rbytes_view(tensor, off_f32, n_elems, dt):
            if dt == F32:
                return tensor[:, off_f32:off_f32 + n_elems]
            assert n_elems % 2 == 0
            return tensor[:, off_f32:off_f32 + n_elems // 2].bitcast(BF16)

        class Arena:
            def __init__(self, tensor, base, size):
                self.t = tensor
                self.base = base
                self.size = size
                self.off = 0

            def reset(self):
                self.off = 0

            def get(self, n_elems, dt, init_r=()):
                words = n_elems if dt == F32 else n_elems // 2
                assert self.off + words <= self.size, ("arena overflow", self.off, words, self.size)
                v = rbytes_view(self.t, self.base + self.off, n_elems, dt)
                self.off += words
                tb = TB(v)
                tb.buf.r = list(init_r)
                return tb

        RA = Arena(R_t, 0, 18432)
        RB = Arena(R_t, 12288, 6144)
        RY = [Arena(R_t, 4096 * i, 4096) for i in range(3)]
        XA = Arena(X_t, 0, 16384)

        P.dma("sp", lambda e: e.dma_start(out=par_t[:, :], in_=par_d[:, :]), "ld_par", writes=[par])
        P.dma("sp", lambda e: e.dma_start(out=cstf_t[:, :], in_=cst_d[:, :]), "ld_cst", writes=[CST])
        for q in range(4):
            P.dma("sp", lambda e, q=q: e.dma_start(
                out=xT.ap[:, q * 4:(q + 1) * 4, :],
                in_=x_d[q * 512:(q + 1) * 512, :].rearrange("(c p) t -> p c t", p=128)),
                "ld_x%d" % q, writes=[xT])
        P.op("dve", lambda v: v.memset(cstb_t[:, 0:128], 1.0), writes=[CST])
        lbl = par_t[:, PO["lbl"]:PO["lbl"] + L * 8].rearrange("p (l h) -> p l h", l=L)
        ex_t = lb_t[:, 0:L * 8].rearrange("p (l h) -> p l h", l=L)
        LB = TB(lb_t[:, :])
        oml_v = lb_t[:, L * 8:2 * L * 8].rearrange("p (l h) -> p l h", l=L)
        ssum = lb_t[:, 2 * L * 8:2 * L * 8 + 8]
        P.op("act", lambda a: a.activation(out=ex_t, in_=lbl, func=AF.Exp), reads=[par], writes=[LB])
        P.op("dve", lambda v: v.tensor_tensor(out=ssum, in0=ex_t[:, 0, :], in1=ex_t[:, 1, :], op=ALU.add), reads=[LB], writes=[LB])
        P.op("dve", lambda v: v.tensor_tensor(out=ssum, in0=ssum, in1=ex_t[:, 2, :], op=ALU.add), reads=[LB], writes=[LB])
        P.op("dve", lambda v: v.tensor_tensor(out=ssum, in0=ssum, in1=ex_t[:, 3, :], op=ALU.add), reads=[LB], writes=[LB])
        P.op("dve", lambda v: v.reciprocal(out=ssum, in_=ssum), reads=[LB], writes=[LB])
        for l in range(L):
            P.op("dve", lambda v, l=l: v.tensor_tensor(out=ex_t[:, l, :], in0=ex_t[:, l, :], in1=ssum, op=ALU.mult), reads=[LB], writes=[LB])
        P.op("dve", lambda v: v.tensor_tensor(out=ex_t[:, 3, :], in0=ex_t[:, 3, :], in1=ex_t[:, 2, :], op=ALU.add), reads=[LB], writes=[LB])
        P.op("dve", lambda v: v.tensor_tensor(out=ex_t[:, 3, :], in0=ex_t[:, 3, :], in1=ex_t[:, 1, :], op=ALU.add), reads=[LB], writes=[LB])
        P.op("dve", lambda v: v.tensor_tensor(out=ex_t[:, 2, :], in0=ex_t[:, 2, :], in1=ex_t[:, 1, :], op=ALU.add), reads=[LB], writes=[LB])
        P.op("dve", lambda v: v.memset(ex_t[:, 0, :], 0.0), reads=[LB], writes=[LB])
        P.op("dve", lambda v: v.tensor_scalar(out=oml_v, in0=ex_t, scalar1=-1.0, scalar2=1.0, op0=ALU.mult, op1=ALU.add), reads=[LB], writes=[LB])
        lb_v = ex_t

        def pcol(name, i):
            return par_t[:, PO[name] + i:PO[name] + i + 1]

        def rms_stats(src_ap_fn, src_buf, nchunks, ntok, inv_n, arena):
            pair = palloc()
            sq = [arena.get(ntok, BF16) for _ in range(2)]
            nt = (ntok + 511) // 512
            w = min(ntok, 512)
            for c in range(nchunks):
                s = sq[c % 2]
                if c % 2 == 0:
                    P.op("act", lambda a, c=c, s=s: a.activation(out=s.ap, in_=src_ap_fn(c), func=AF.Square),
                         reads=[src_buf], writes=[s])
                else:
                    P.op("dve", lambda v, c=c, s=s: v.tensor_tensor(out=s.ap, in0=src_ap_fn(c), in1=src_ap_fn(c), op=ALU.mult),
                         reads=[src_buf], writes=[s])
                P.group("pe", [
                    (lambda t, c=c, s=s, tt=tt: t.matmul(pair.ap[:, tt * 512:tt * 512 + w], lhsT=ones_b,
                                                        rhs=s.ap[:, tt * w:(tt + 1) * w],
                                                        start=(c == 0), stop=(c == nchunks - 1)))
                    for tt in range(nt)], reads=[s, CST], writes=[pair])
            rstd = arena.get(ntok, F32)
            if nt == 2:
                pv = pair.ap[:, 0:ntok]
            else:
                pv = pair.ap[:, 0:w]
            P.op("act", lambda a: a.activation(out=rstd.ap, in_=pv, func=AF.Ln, bias=epsb, scale=inv_n),
                 reads=[pair, CST], writes=[rstd])
            P.op("act", lambda a: a.activation(out=rstd.ap, in_=rstd.ap, func=AF.Exp, scale=-0.5),
                 reads=[rstd], writes=[rstd])
            return rstd

        epsb = lb_t[:, 2 * L * 8 + 8:2 * L * 8 + 9]
        P.op("dve", lambda v: v.memset(epsb, EPS), writes=[CST])

        class FusedStats:
            def __init__(self, arena):
                self.pair = palloc()
                self.k = pairs.index(self.pair)
                reserved.add(self.k)
                self.sq = [arena.get(T, BF16) for _ in range(4)]
                self.q = []
                self.n = 0

            def _mm(self, i, s_):
                pair = self.pair
                P.group("pe", [
                    (lambda t, tt=tt: t.matmul(pair.ap[:, tt * 512:(tt + 1) * 512], lhsT=ones_b,
                                               rhs=s_.ap[:, tt * 512:(tt + 1) * 512], start=(i == 0), stop=(i == KC - 1)))
                    for tt in range(2)], reads=[s_, CST], writes=[pair])

            def chunk(self, c):
                s_ = self.sq[self.n % 4]
                P.op("act", lambda a: a.activation(out=s_.ap, in_=xT.ap[:, c, :], func=AF.Square), reads=[xT], writes=[s_])
                self.q.append((self.n, s_))
                self.n += 1
                if len(self.q) > 2:
                    self._mm(*self.q.pop(0))

            def flush(self):
                while self.q:
                    self._mm(*self.q.pop(0))

        def x_rstd(arena):
            fs = pend.pop("fs", None)
            if fs is None:
                return rms_stats(lambda c: xT.ap[:, c, :], xT, KC, T, 1.0 / D, arena)
            assert fs.n == KC
            rstd = arena.get(T, F32)
            pair = fs.pair
            P.op("act", lambda a: a.activation(out=rstd.ap, in_=pair.ap, func=AF.Ln, bias=epsb, scale=1.0 / D),
                 reads=[pair, CST], writes=[rstd])
            P.op("act", lambda a: a.activation(out=rstd.ap, in_=rstd.ap, func=AF.Exp, scale=-0.5),
                 reads=[rstd], writes=[rstd])
            reserved.discard(fs.k)
            return rstd

        def norm_to_hT(gname, l, arena):
            rstd = x_rstd(arena)
            for c in range(KC):
                P.op("dve", lambda v, c=c: v.scalar_tensor_tensor(
                    out=hT.ap[:, c, :], in0=xT.ap[:, c, :], scalar=pcol(gname, l * 16 + c), in1=rstd.ap,
                    op0=ALU.mult, op1=ALU.mult), reads=[xT, rstd, par], writes=[hT])

        def ffn(l, which):
            RA.reset()
            P.barrier()
            norm_to_hT("n1" if which == 1 else "n2", l, RA)
            hid = RA.get(GJ * T, BF16)
            hv = hid.ap.rearrange("p (j t) -> p j t", j=GJ)
            sg = [RA.get(T, F32) for _ in range(2)]
            gu = "gu%d" % which
            dn = "d%d" % which
            for g in range(NG):
                for j in range(GJ):
                    f = g * GJ + j
                    s = wload(gu, LI[l] * NF + f, 4096)
                    wv = s.ap.rearrange("p (g k m) -> p g k m", g=2, k=KC)
                    pp = []
                    for q in range(2):
                        pair = palloc()
                        pp.append(pair)
                        P.group("pe", [
                            (lambda t, q=q, kc=kc, tt=tt, pair=pair, wv=wv: t.matmul(
                                pair.ap[:, tt * 512:(tt + 1) * 512], lhsT=wv[:, q, kc, :],
                                rhs=hT.ap[:, kc, tt * 512:(tt + 1) * 512], start=(kc == 0), stop=(kc == KC - 1)))
                            for kc in range(KC) for tt in range(2)], reads=[s, hT], writes=[pair])
                    st = sg[f % 2]
                    P.op("act", lambda a, st=st, pg=pp[0]: a.activation(out=st.ap, in_=pg.ap, func=AF.Silu),
                         reads=[pp[0]], writes=[st])
                    P.op("dve", lambda v, st=st, pu=pp[1], j=j: v.tensor_tensor(
                        out=hv[:, j, :], in0=st.ap, in1=pu.ap, op=ALU.mult), reads=[st, pp[1]], writes=[hid])
                fs = FusedStats(RA) if g == NG - 1 else None
                for dcp in range(8):
                    s = wload(dn, (LI[l] * NG + g) * 8 + dcp, 2816)
                    wv = s.ap[:, 0:2816].rearrange("p (d j m) -> p d j m", d=2, j=GJ)
                    for d2 in range(2):
                        dc = dcp * 2 + d2
                        pair = palloc()
                        P.group("pe", [
                            (lambda t, d2=d2, j=j, tt=tt, pair=pair, wv=wv: t.matmul(
                                pair.ap[:, tt * 512:(tt + 1) * 512], lhsT=wv[:, d2, j, :],
                                rhs=hv[:, j, tt * 512:(tt + 1) * 512], start=(j == 0), stop=(j == GJ - 1)))
                            for j in range(GJ) for tt in range(2)], reads=[s, hid], writes=[pair])
                        P.op("dve", lambda v, dc=dc, pair=pair: v.scalar_tensor_tensor(
                            out=xT.ap[:, dc, :], in0=pair.ap, scalar=0.5, in1=xT.ap[:, dc, :],
                            op0=ALU.mult, op1=ALU.add), reads=[pair, xT], writes=[xT])
                        if fs is not None:
                            fs.chunk(dc)
                if fs is not None:
                    fs.flush()
                    pend["fs"] = fs

        cmask8 = cstf[:, 1152:1664].rearrange("p (b t) -> p b t", b=8)
        ones16 = cstf[:, 1664:1680]
        rg_pairs = [[2 * i, 2 * i + 1] for i in range(ncores // 2)]

        def mm32(pair, lhs_fn, reads):
            return P.group("pe", [
                (lambda t, kc=kc, tt=tt: t.matmul(pair.ap[:, tt * 512:(tt + 1) * 512], lhsT=lhs_fn(kc),
                                                  rhs=hT.ap[:, kc, tt * 512:(tt + 1) * 512],
                                                  start=(kc == 0), stop=(kc == KC - 1)))
                for kc in range(KC) for tt in range(2)], reads=list(reads) + [hT], writes=[pair])

        def mixer(l):
            li = LI[l]
            RA.reset()
            P.barrier()
            norm_to_hT("nm", l, RA)
            spill = []
            for q in range(4):
                spill.append(P.dma("sp", lambda e, q=q: e.dma_start(
                    out=xsp_d[q * 512:(q + 1) * 512, :].rearrange("(c p) t -> p c t", p=128),
                    in_=xT.ap[:, q * 4:(q + 1) * 4, :]), "sp_x%d" % q, reads=[xT]))
            P.barrier()

            def reload_x():
                P.barrier()
                bt = [(e, P.E[e].cnt) for e in ("pe", "act", "dve")]
                for q in range(4):
                    P.dma("sp", lambda e, q=q: e.dma_start(
                        out=xT.ap[:, q * 4:(q + 1) * 4, :],
                        in_=xsp_d[q * 512:(q + 1) * 512, :].rearrange("(c p) t -> p c t", p=128)),
                        "ld_x%d" % q, writes=[xT], extra=bt + spill)
            if MIXCUT == 0:
                reload_x()
                return
            RA.reset()
            XA.reset()
            Vt = RA.get(8192, BF16)
            Vv = Vt.ap.rearrange("p (b c) -> p b c", b=8)
            A, B, C, Dd, E, Q = [RA.get(1024, F32) for _ in range(6)]
            Qt, Kb, Qh = [RA.get(1024, BF16) for _ in range(3)]
            Kp = RA.get(2048, BF16)
            Kp4 = Kp.ap.rearrange("p (b two d) -> p b two d", two=2, d=128)
            Kp3 = Kp.ap.rearrange("p (c d) -> p c d", d=128)
            Sm = RA.get(1024, BF16)
            Sm4 = Sm.ap.rearrange("p (b two t) -> p b two t", two=2, t=64)
            Sm3 = Sm.ap.rearrange("p (c t) -> p c t", t=64)
            Sall = RA.get(2048, F32)
            Sall3 = Sall.ap.rearrange("p (c d) -> p c d", d=128)
            Sb = RA.get(2048, BF16)
            Sb3 = Sb.ap.rearrange("p (c d) -> p c d", d=128)
            sm = RA.get(128, F32)
            dLm, eLm, aC, BcI, Bx = [sm.ap[:, i * 16:(i + 1) * 16] for i in range(5)]
            Qff = XA.get(8192, BF16, spill)
            Qff3 = Qff.ap.rearrange("p (h t) -> p h t", h=8)
            oloc = XA.get(8192, F32, spill)
            oloc3 = oloc.ap.rearrange("p (h t) -> p h t", h=8)
            sendb = XA.get(1152, F32, spill)
            recvb = XA.get(1152, F32, spill)
            Sin = XA.get(1024, BF16, spill)
            Sin3 = Sin.ap.rearrange("p (h d) -> p h d", h=8)
            xsm = XA.get(640, F32, spill)
            uprev = xsm.ap[:, 0:128]
            cpre = xsm.ap[:, 128:256]
            cbs = xsm.ap[:, 256:384]
            t1s = xsm.ap[:, 384:400]
            t2s = xsm.ap[:, 400:416]
            yfix = xsm.ap[:, 416:432]
            mask10 = cstf[:, 1680:1696].rearrange("p (j k) -> p j k", k=2)
            P.op("dve", lambda v: v.memset(Kp.ap, 0.0), writes=[Kp])
            P.op("dve", lambda v: v.memset(Sm.ap, 0.0), writes=[Sm])
            for hp in range(4):
                s = wload("vi", li * 4 + hp, 4096)
                wv = s.ap.rearrange("p (k n) -> p k n", k=KC)
                for tbp in range(2):
                    pair = palloc()
                    P.group("pe", [
                        (lambda t, tbi=tbi, kc=kc, pair=pair, wv=wv, tbp=tbp: t.matmul(
                            pair.ap[:, tbi * 256:(tbi + 1) * 256],
                            lhsT=hT.ap[:, kc, (tbp * 4 + tbi) * 128:(tbp * 4 + tbi + 1) * 128],
                            rhs=wv[:, kc, :], start=(kc == 0), stop=(kc == KC - 1)))
                        for tbi in range(4) for kc in range(KC)], reads=[s, hT], writes=[pair])
                    P.op("act", lambda a, pair=pair, tbp=tbp, hp=hp: a.activation(
                        out=Vv[:, tbp * 4:(tbp + 1) * 4, hp * 256:(hp + 1) * 256],
                        in_=pair.ap.rearrange("p (b n) -> p b n", b=4), func=AF.Copy), reads=[pair], writes=[Vt])
            def halves(tb):
                return [TB(tb.ap[:, 0:512]), TB(tb.ap[:, 512:1024])]

            def v3(tb):
                return tb.ap.rearrange("p (c t) -> p c t", t=64)

            Ah, Bh, Ch, Dh, Eh, Qs, Qth, Kbh, Qhh = [halves(x_) for x_ in (A, B, C, Dd, E, Q, Qt, Kb, Qh)]
            C3 = C.ap.rearrange("p (c t) -> p c t", t=64)
            A3 = A.ap.rearrange("p (c t) -> p c t", t=64)
            B3 = B.ap.rearrange("p (c t) -> p c t", t=64)
            D3 = Dd.ap.rearrange("p (c t) -> p c t", t=64)
            tstate = {"i": 0}

            def talloc():
                p_ = pairs[2 + tstate["i"] % 2]
                tstate["i"] += 1
                return p_

            Qt_b, Kb_b, Qh_b = [RA.get(1024, BF16) for _ in range(3)]
            sm_b = RA.get(128, F32)
            Kp_b = TB(recvb.ap[:, 0:1024].bitcast(BF16))
            Kp_b.buf.r = list(spill)
            P.op("dve", lambda v: v.memset(Kp_b.ap, 0.0), writes=[Kp_b])
            PAR = []
            for (qt_, kb_, qh_, kp_, sm_) in ((Qt, Kb, Qh, Kp, sm), (Qt_b, Kb_b, Qh_b, Kp_b, sm_b)):
                d_ = {"Qth": halves(qt_), "Kbh": halves(kb_), "Qhh": halves(qh_), "Kp": kp_,
                      "Kp4": kp_.ap.rearrange("p (b two d) -> p b two d", two=2, d=128),
                      "Kp3": kp_.ap.rearrange("p (c d) -> p c d", d=128),
                      "Qt": qt_, "Kb": kb_, "Qh": qh_, "sm": sm_}
                for i_, nm_ in enumerate(("dLm", "eLm", "aC", "BcI", "Bx")):
                    d_[nm_] = sm_.ap[:, i_ * 16:(i_ + 1) * 16]
                PAR.append(d_)

            def emit_proj(h_):
                s_ = wload("qf", li * 8 + h_, 4096)
                wv_ = s_.ap.rearrange("p (g k m) -> p g k m", g=2, k=KC)
                mm32(pairs[0], lambda kc, wv_=wv_: wv_[:, 0, kc, :], [s_])
                mm32(pairs[1], lambda kc, wv_=wv_: wv_[:, 1, kc, :], [s_])

            def part1(h):
                pr = PAR[h % 2]
                Qth, Kbh, Qhh = pr["Qth"], pr["Kbh"], pr["Qhh"]
                Kp4_, smb = pr["Kp4"], pr["sm"]
                dLm, eLm, aC, BcI, Bx = pr["dLm"], pr["eLm"], pr["aC"], pr["BcI"], pr["Bx"]
                pq = pairs[0]
                pz = pairs[1]
                lbc = lb_v[:, l, h:h + 1]
                omlc = oml_v[:, l, h:h + 1]
                H2 = (0, 1)
                for tt in H2:
                    P.op("act", lambda a, tt=tt: a.activation(out=Ah[tt].ap, in_=pz.ap[:, tt * 512:(tt + 1) * 512], func=AF.Sigmoid), reads=[pz], writes=[Ah[tt]])
                    yield
                for tt in H2:
                    P.op("act", lambda a, tt=tt: a.activation(out=Bh[tt].ap, in_=pz.ap[:, tt * 512:(tt + 1) * 512], func=AF.Sigmoid, scale=-1.0), reads=[pz], writes=[Bh[tt]])
                    yield
                for tt in H2:
                    P.op("act", lambda a, tt=tt: a.activation(out=Qs[tt].ap, in_=pq.ap[:, tt * 512:(tt + 1) * 512], func=AF.Copy), reads=[pq], writes=[Qs[tt]])
                    yield
                if h + 1 < 8:
                    emit_proj(h + 1)
                    yield
                for tt in H2:
                    P.op("dve", lambda v, tt=tt: v.tensor_scalar(out=Ah[tt].ap, in0=Ah[tt].ap, scalar1=omlc, scalar2=lbc, op0=ALU.mult, op1=ALU.add), reads=[Ah[tt], LB], writes=[Ah[tt]])
                    yield
                for tt in H2:
                    P.op("dve", lambda v, tt=tt: v.tensor_scalar(out=Ah[tt].ap, in0=Ah[tt].ap, scalar1=1e-30, scalar2=None, op0=ALU.max), reads=[Ah[tt]], writes=[Ah[tt]])
                    yield
                for tt in H2:
                    P.op("act", lambda a, tt=tt: a.activation(out=Ah[tt].ap, in_=Ah[tt].ap, func=AF.Ln), reads=[Ah[tt]], writes=[Ah[tt]])
                    yield
                for tt in H2:
                    P.op("dve", lambda v, tt=tt: v.tensor_tensor_scan(out=Ch[tt].ap, data0=rmask[:, tt * 512:(tt + 1) * 512], data1=Ah[tt].ap, initial=0.0, op0=ALU.mult, op1=ALU.add), reads=[Ah[tt], CST], writes=[Ch[tt]])
                    yield
                for tt in H2:
                    P.op("dve", lambda v, tt=tt: v.tensor_tensor(out=v3(Dh[tt]), in0=v3(Ch[tt]), in1=v3(Ch[tt])[:, :, 31:32].broadcast_to([128, 8, 64]), op=ALU.subtract), reads=[Ch[tt]], writes=[Dh[tt]])
                    yield
                for tt in H2:
                    P.op("act", lambda a, tt=tt: a.activation(out=Eh[tt].ap, in_=Dh[tt].ap, func=AF.Exp), reads=[Dh[tt]], writes=[Eh[tt]])
                    yield
                for tt in H2:
                    P.op("dve", lambda v, tt=tt: v.tensor_tensor(out=Qth[tt].ap, in0=Qs[tt].ap, in1=Eh[tt].ap, op=ALU.mult), reads=[Qs[tt], Eh[tt]], writes=[Qth[tt]])
                    yield
                for tt in H2:
                    P.op("act", lambda a, tt=tt: a.activation(out=Eh[tt].ap, in_=Dh[tt].ap, func=AF.Exp, scale=-1.0), reads=[Dh[tt]], writes=[Eh[tt]])
                    yield
                for tt in H2:
                    P.op("dve", lambda v, tt=tt: v.scalar_tensor_tensor(out=Bh[tt].ap, in0=Bh[tt].ap, scalar=omlc, in1=Eh[tt].ap, op0=ALU.mult, op1=ALU.mult), reads=[Bh[tt], Eh[tt], LB], writes=[Bh[tt]])
                    yield
                for tt in H2:
                    P.op("act", lambda a, tt=tt: a.activation(out=Kbh[tt].ap, in_=Bh[tt].ap, func=AF.Copy), reads=[Bh[tt]], writes=[Kbh[tt]])
                    yield
                P.op("dve", lambda v: v.tensor_tensor(out=dLm, in0=C3[:, :, 63], in1=C3[:, :, 31], op=ALU.subtract), reads=Ch, writes=[smb])
                yield
                P.op("act", lambda a: a.activation(out=eLm, in_=dLm, func=AF.Exp), reads=[smb], writes=[smb])
                yield
                P.op("act", lambda a: a.activation(out=aC, in_=C3[:, :, 63], func=AF.Exp), reads=Ch + [smb], writes=[smb])
                yield
                for tt in H2:
                    P.op("dve", lambda v, tt=tt: v.tensor_tensor(out=v3(Dh[tt]), in0=v3(Bh[tt]), in1=eLm[:, tt * 8:(tt + 1) * 8].unsqueeze(2).broadcast_to([128, 8, 64]), op=ALU.mult), reads=[Bh[tt], smb], writes=[Dh[tt]])
                    yield
                ptr = talloc()
                P.group("pe", [
                    (lambda t, tb=tb: t.transpose(out=ptr.ap[:, tb * 128:(tb + 1) * 128], in_=Dd.ap[:, tb * 128:(tb + 1) * 128], identity=ident))
                    for tb in range(8)], reads=Dh + [CST], writes=[ptr])
                yield
                ptr3 = ptr.ap.rearrange("p (b d) -> p b d", d=128)
                P.op("act", lambda a: a.activation(out=Kp4_[0:64, :, 0, :], in_=ptr3[0:64, :, :], func=AF.Copy), reads=[ptr], writes=[pr["Kp"]])
                yield
                P.op("act", lambda a: a.activation(out=Kp4_[64:128, :, 1, :], in_=ptr3[64:128, :, :], func=AF.Copy), reads=[ptr], writes=[pr["Kp"]])
                yield
                for tt in H2:
                    P.op("act", lambda a, tt=tt: a.activation(out=Eh[tt].ap, in_=Ch[tt].ap, func=AF.Exp), reads=[Ch[tt]], writes=[Eh[tt]])
                    yield
                for tt in H2:
                    P.op("dve", lambda v, tt=tt: v.tensor_tensor(out=Qhh[tt].ap, in0=Qs[tt].ap, in1=Eh[tt].ap, op=ALU.mult), reads=[Qs[tt], Eh[tt]], writes=[Qhh[tt]])
                    yield
                P.op("dve", lambda v: v.tensor_tensor_scan(out=BcI, data0=ones16, data1=C3[:, :, 63], initial=0.0, op0=ALU.mult, op1=ALU.add), reads=Ch + [CST, smb], writes=[smb])
                yield
                P.op("dve", lambda v: v.tensor_tensor(out=Bx, in0=BcI, in1=C3[:, :, 63], op=ALU.subtract), reads=Ch + [smb], writes=[smb])
                yield
                for tt in H2:
                    P.op("dve", lambda v, tt=tt: v.tensor_tensor(out=v3(Ah[tt]), in0=v3(Ch[tt]), in1=Bx[:, tt * 8:(tt + 1) * 8].unsqueeze(2).broadcast_to([128, 8, 64]), op=ALU.add), reads=[Ch[tt], smb], writes=[Ah[tt]])
                    yield
                for tt in H2:
                    P.op("act", lambda a, tt=tt: a.activation(out=Eh[tt].ap, in_=Ah[tt].ap, func=AF.Exp), reads=[Ah[tt]], writes=[Eh[tt]])
                    yield
                for tt in H2:
                    P.op("dve", lambda v, tt=tt: v.tensor_tensor(out=Qff3[:, h, tt * 512:(tt + 1) * 512], in0=Qs[tt].ap, in1=Eh[tt].ap, op=ALU.mult), reads=[Qs[tt], Eh[tt]], writes=[Qff])
                    yield

            def tail(h):
                pr = PAR[h % 2]
                Qth, Kbh, Qhh = pr["Qth"], pr["Kbh"], pr["Qhh"]
                Kp3_, smb, aC = pr["Kp3"], pr["sm"], pr["aC"]
                Kb_, Qt_, Qh_ = pr["Kb"], pr["Qt"], pr["Qh"]
                pU = [talloc(), talloc()]
                P.group("pe", [
                    (lambda t, c=c: t.matmul(pU[c // 8].ap[:, (c % 8) * 128:(c % 8 + 1) * 128], lhsT=Kp3_[:, c, :],
                                             rhs=Vv[:, c // 2, h * 128:(h + 1) * 128], start=True, stop=True))
                    for c in range(16)], reads=[pr["Kp"], Vt], writes=pU)
                yield
                P.op("dve", lambda v: v.tensor_copy(out=Sall3[:, 0, :], in_=pU[0].ap[:, 0:128]), reads=[pU[0]], writes=[Sall])
                yield
                for c in range(1, 16):
                    P.op("dve", lambda v, c=c: v.scalar_tensor_tensor(
                        out=Sall3[:, c, :], in0=Sall3[:, c - 1, :], scalar=aC[:, c:c + 1],
                        in1=pU[c // 8].ap[:, (c % 8) * 128:(c % 8 + 1) * 128], op0=ALU.mult, op1=ALU.add),
                        reads=[Sall, smb, pU[c // 8]], writes=[Sall])
                    yield
                P.op("act", lambda a: a.activation(out=Sb.ap, in_=Sall.ap, func=AF.Copy), reads=[Sall], writes=[Sb])
                yield
                P.op("act", lambda a: a.activation(out=sendb.ap[:, h * 128:(h + 1) * 128], in_=Sall3[:, 15, :], func=AF.Copy), reads=[Sall], writes=[sendb])
                yield
                pS = talloc()
                P.group("pe", [
                    (lambda t, c=c: t.matmul(pS.ap[(c % 2) * 64:(c % 2) * 64 + 64, (c // 2) * 64:(c // 2) * 64 + 64],
                                             lhsT=Kb_.ap[:, c * 64:(c + 1) * 64], rhs=Qt_.ap[:, c * 64:(c + 1) * 64],
                                             start=True, stop=True))
                    for c in range(16)], reads=Kbh + Qth, writes=[pS])
                yield
                pS3 = pS.ap[:, 0:512].rearrange("p (b t) -> p b t", t=64)
                P.op("dve", lambda v: v.tensor_tensor(out=Sm4[0:64, :, 0, :], in0=pS3[0:64, :, :], in1=cmask8[0:64, :, :], op=ALU.mult), reads=[pS, CST], writes=[Sm])
                yield
                P.op("dve", lambda v: v.tensor_tensor(out=Sm4[64:128, :, 1, :], in0=pS3[64:128, :, :], in1=cmask8[64:128, :, :], op=ALU.mult), reads=[pS, CST], writes=[Sm])
                yield
                pO = talloc()
                fns = []
                for c in range(16):
                    fns.append(lambda t, c=c: t.matmul(pO.ap[:, c * 64:(c + 1) * 64], lhsT=Vv[:, c // 2, h * 128:(h + 1) * 128],
                                                       rhs=Sm3[:, c, :], start=True, stop=(c == 0)))
                    if c > 0:
                        fns.append(lambda t, c=c: t.matmul(pO.ap[:, c * 64:(c + 1) * 64], lhsT=Sb3[:, c - 1, :],
                                                           rhs=Qh_.ap[:, c * 64:(c + 1) * 64], start=False, stop=True))
                P.group("pe", fns, reads=[Vt, Sm, Sb] + Qhh, writes=[pO])
                yield
                P.op("act", lambda a: a.activation(out=oloc3[:, h, :], in_=pO.ap, func=AF.Copy), reads=[pO], writes=[oloc])
                yield

            def drain(*gens):
                gens = list(gens)
                while gens:
                    for g_ in list(gens):
                        try:
                            next(g_)
                        except StopIteration:
                            gens.remove(g_)

            emit_proj(0)
            drain(part1(0))
            for h in range(8):
                if h + 1 < 8:
                    drain(part1(h + 1), tail(h))
                else:
                    drain(tail(h))
            if MIXCUT == 1:
                reload_x()
                return
            P.barrier()
            RB.reset()
            for a_ in RY:
                a_.reset()
            yconv = RY[0].get(8192, BF16)
            yh = RY[1]
            ymem_a = RY[2]
            yc3 = yconv.ap.rearrange("p (j t) -> p j t", j=8)
            xs = RB.get(1024, F32)
            ub = RB.get(1040, F32)
            acc = RB.get(1024, F32)
            P.op("dve", lambda v: v.memset(ub.ap[:, 0:16], 0.0), writes=[ub])
            for j in range(8):
                s1 = wload("cxc", li * 8 + j, 4096)
                wv1 = s1.ap.rearrange("p (g k m) -> p g k m", g=2, k=KC)
                s2 = wload("cb", li * 8 + j, 2048)
                wv2 = s2.ap[:, 0:2048].rearrange("p (k m) -> p k m", k=KC)
                pX = palloc()
                mm32(pX, lambda kc, wv1=wv1: wv1[:, 0, kc, :], [s1])
                pC = palloc()
                mm32(pC, lambda kc, wv1=wv1: wv1[:, 1, kc, :], [s1])
                pB = palloc()
                mm32(pB, lambda kc, wv2=wv2: wv2[:, kc, :], [s2])
                w0 = pcol("cw", l * 24 + 0 * 8 + j)
                w1 = pcol("cw", l * 24 + 1 * 8 + j)
                w2 = pcol("cw", l * 24 + 2 * 8 + j)
                P.op("act", lambda a, pX=pX: a.activation(out=xs.ap, in_=pX.ap, func=AF.Copy), reads=[pX], writes=[xs])
                P.op("dve", lambda v, pC=pC: v.tensor_tensor(out=ub.ap[:, 16:1040], in0=pC.ap, in1=xs.ap, op=ALU.mult), reads=[pC, xs], writes=[ub])
                if CONVDBG >= 2:
                    P.op("dve", lambda v, j=j, pB=pB: v.tensor_tensor(out=yc3[:, j, :], in0=pB.ap, in1=ub.ap[:, 16:1040], op=ALU.mult), reads=[pB, ub], writes=[yconv])
                    continue
                P.op("act", lambda a, w1=w1: a.activation(out=xs.ap, in_=ub.ap[:, 15:1039], func=AF.Identity, scale=w1), reads=[ub, par], writes=[xs])
                P.op("dve", lambda v, w2=w2: v.scalar_tensor_tensor(out=acc.ap, in0=ub.ap[:, 16:1040], scalar=w2, in1=xs.ap, op0=ALU.mult, op1=ALU.add), reads=[ub, par, xs], writes=[acc])
                P.op("dve", lambda v, w0=w0: v.scalar_tensor_tensor(out=acc.ap, in0=ub.ap[:, 14:1038], scalar=w0, in1=acc.ap, op0=ALU.mult, op1=ALU.add), reads=[ub, par, acc], writes=[acc])
                P.op("dve", lambda v, j=j, pB=pB: v.tensor_tensor(out=yc3[:, j, :], in0=pB.ap, in1=acc.ap, op=ALU.mult), reads=[pB, acc], writes=[yconv])
                if CONVDBG >= 1:
                    continue
                P.op("dve", lambda v, j=j: v.tensor_copy(out=sendb.ap[:, 1024 + 16 * j:1040 + 16 * j], in_=ub.ap[:, 1024:1040]), reads=[ub], writes=[sendb])
                P.op("dve", lambda v, j=j: v.tensor_copy(out=cpre[:, 16 * j:16 * j + 16], in_=acc.ap[:, 0:16]), reads=[acc], writes=[xsm])
                P.op("dve", lambda v, j=j, pB=pB: v.tensor_copy(out=cbs[:, 16 * j:16 * j + 16], in_=pB.ap[:, 0:16]), reads=[pB], writes=[xsm])
            if MIXCUT == 2:
                reload_x()
                return
            t_s = P.dma("pool", lambda g: g.dma_start(out=ccs_d[:, :], in_=sendb.ap), "cc_s", reads=[sendb])
            t_c = P.custom("pool", lambda g: g.collective_compute("AllGather", ALU.bypass, replica_groups=rg_pairs,
                                                                  ins=[ccs_d[:, :]], outs=[ccd_d[:, :]]), "cc_c", 1, extra=[t_s])
            P.dma("pool", lambda g: g.dma_start(out=recvb.ap, in_=ccd_d[0:128, :]), "cc_r", writes=[recvb], extra=[t_c])
            if MIXCUT == 3:
                P.wait("dve", [P.dcnt and ("cc_r", P.dcnt["cc_r"])])
                reload_x()
                return
            P.barrier()
            RB.reset()
            RY[1].reset()
            memraw = RB.get(4096, F32)
            mr3 = memraw.ap.rearrange("p (c m) -> p c m", c=KC)
            memn = RB.get(4096, BF16)
            mn3 = memn.ap.rearrange("p (c m) -> p c m", c=KC)
            kT = RY[1].get(2048, BF16)
            kT3 = kT.ap.rearrange("p (j m) -> p j m", j=8)
            vM = RY[1].get(2048, BF16)
            vM3 = vM.ap.rearrange("p (b n) -> p b n", b=2)
            bt3 = [(e, P.E[e].cnt) for e in ("pe", "act", "dve")]
            for q in range(4):
                P.dma("sp", lambda e, q=q: e.dma_start(out=mr3[:, q * 4:(q + 1) * 4, :],
                                                       in_=mem_d[q * 512:(q + 1) * 512, :].rearrange("(c p) m -> p c m", p=128)),
                      "ld_mem%d" % q, writes=[memraw], extra=bt3)
            rstd = rms_stats(lambda c: mr3[:, c, :], memraw, KC, 256, 1.0 / D, RY[1])
            for c in range(KC):
                P.op("dve", lambda v, c=c: v.scalar_tensor_tensor(out=mn3[:, c, :], in0=mr3[:, c, :], scalar=pcol("nmem", l * 16 + c),
                                                                  in1=rstd.ap, op0=ALU.mult, op1=ALU.mult), reads=[memraw, rstd, par], writes=[memn])
            for jp in range(4):
                s = wload("kk", li * 4 + jp, 4096)
                wv = s.ap.rearrange("p (g k m) -> p g k m", g=2, k=KC)
                pair = palloc()
                P.group("pe", [
                    (lambda t, j2=j2, kc=kc, pair=pair, wv=wv: t.matmul(pair.ap[:, j2 * 256:(j2 + 1) * 256], lhsT=wv[:, j2, kc, :],
                                                                       rhs=mn3[:, kc, :], start=(kc == 0), stop=(kc == KC - 1)))
                    for j2 in range(2) for kc in range(KC)], reads=[s, memn], writes=[pair])
                P.op("act", lambda a, jp=jp, pair=pair: a.activation(out=kT3[:, jp * 2:(jp + 1) * 2, :],
                                                                     in_=pair.ap[:, 0:512].rearrange("p (j m) -> p j m", j=2), func=AF.Copy),
                     reads=[pair], writes=[kT])
            for vq in range(4):
                s = wload("kv", li * 4 + vq, 4096)
                wv = s.ap.rearrange("p (k n) -> p k n", k=KC)
                pair = palloc()
                P.group("pe", [
                    (lambda t, mb=mb, kc=kc, pair=pair, wv=wv: t.matmul(pair.ap[:, mb * 256:(mb + 1) * 256],
                                                                       lhsT=mn3[:, kc, mb * 128:(mb + 1) * 128],
                                                                       rhs=wv[:, kc, :], start=(kc == 0), stop=(kc == KC - 1)))
                    for mb in range(2) for kc in range(KC)], reads=[s, memn], writes=[pair])
                P.op("act", lambda a, vq=vq, pair=pair: a.activation(out=vM3[:, :, vq * 256:(vq + 1) * 256],
                                                                     in_=pair.ap[:, 0:512].rearrange("p (b n) -> p b n", b=2), func=AF.Copy),
                     reads=[pair], writes=[vM])
            P.barrier()
            RB.reset()
            ymem = RY[2].get(8192, BF16)
            ym3 = ymem.ap.rearrange("p (j t) -> p j t", j=8)
            qa = RB.get(2048, BF16)
            qa3 = qa.ap.rearrange("p (d t) -> p d t", d=2)
            Eb = RB.get(2048, BF16)
            Eb3 = Eb.ap.rearrange("p (b t) -> p b t", b=2)
            rden = RB.get(1024, F32)
            for a_ in range(4):
                s = wload("aq", li * 4 + a_, 4096)
                wv = s.ap.rearrange("p (g k m) -> p g k m", g=2, k=KC)
                for d2 in range(2):
                    pair = palloc()
                    mm32(pair, lambda kc, wv=wv, d2=d2: wv[:, d2, kc, :], [s])
                    P.op("act", lambda a, d2=d2, pair=pair: a.activation(out=qa3[:, d2, :], in_=pair.ap, func=AF.Copy), reads=[pair], writes=[qa])
                for mb in range(2):
                    pair = palloc()
                    P.group("pe", [
                        (lambda t, tt=tt, d2=d2, mb=mb, a_=a_, pair=pair: t.matmul(
                            pair.ap[:, tt * 512:(tt + 1) * 512], lhsT=kT3[:, 2 * a_ + d2, mb * 128:(mb + 1) * 128],
                            rhs=qa3[:, d2, tt * 512:(tt + 1) * 512], start=(d2 == 0), stop=(d2 == 1)))
                        for tt in range(2) for d2 in range(2)], reads=[kT, qa], writes=[pair])
                    P.op("act", lambda a, mb=mb, pair=pair: a.activation(out=Eb3[:, mb, :], in_=pair.ap, func=AF.Exp, scale=1.0 / 16.0),
                         reads=[pair], writes=[Eb])
                pair = palloc()
                P.group("pe", [
                    (lambda t, tt=tt, mb=mb, pair=pair: t.matmul(pair.ap[:, tt * 512:(tt + 1) * 512], lhsT=ones_b,
                                                                rhs=Eb3[:, mb, tt * 512:(tt + 1) * 512], start=(mb == 0), stop=(mb == 1)))
                    for tt in range(2) for mb in range(2)], reads=[Eb, CST], writes=[pair])
                P.op("dve", lambda v, pair=pair: v.reciprocal(out=rden.ap, in_=pair.ap), reads=[pair], writes=[rden])
                for dv2 in range(2):
                    pair = palloc()
                    P.group("pe", [
                        (lambda t, tt=tt, mb=mb, a_=a_, dv2=dv2, pair=pair: t.matmul(
                            pair.ap[:, tt * 512:(tt + 1) * 512], lhsT=vM3[:, mb, a_ * 256 + dv2 * 128:a_ * 256 + dv2 * 128 + 128],
                            rhs=Eb3[:, mb, tt * 512:(tt + 1) * 512], start=(mb == 0), stop=(mb == 1)))
                        for tt in range(2) for mb in range(2)], reads=[vM, Eb], writes=[pair])
                    P.op("dve", lambda v, a_=a_, dv2=dv2, pair=pair: v.tensor_tensor(out=ym3[:, 2 * a_ + dv2, :], in0=pair.ap, in1=rden.ap, op=ALU.mult),
                         reads=[pair, rden], writes=[ymem])
            if MIXCUT == 4:
                P.wait("dve", [("cc_r", P.dcnt["cc_r"])])
                reload_x()
                return
            P.barrier()
            RB.reset()
            RY[1].reset()
            yhb = RY[1].get(8192, BF16)
            yh3 = yhb.ap.rearrange("p (h t) -> p h t", h=8)
            FS = [{"sq": RB.get(1024, BF16), "rs": RB.get(1024, F32), "sg": RB.get(1024, F32)} for _ in range(2)]
            ol = [TB(oloc3[:, h_, :]) for h_ in range(8)]
            flag = pcol("flag", 0)
            P.op("dve", lambda v: v.tensor_scalar(out=Sin.ap, in0=recvb.ap[:, 0:1024], scalar1=flag, scalar2=None, op0=ALU.mult), reads=[recvb, par], writes=[Sin])
            P.op("dve", lambda v: v.tensor_scalar(out=uprev, in0=recvb.ap[:, 1024:1152], scalar1=flag, scalar2=None, op0=ALU.mult), reads=[recvb, par], writes=[xsm])

            def fin(h, fs):
                sq, rs, sg = fs["sq"], fs["rs"], fs["sg"]
                pair = palloc()
                P.group("pe", [
                    (lambda t, tt=tt: t.matmul(pair.ap[:, tt * 512:(tt + 1) * 512], lhsT=Sin3[:, h, :],
                                               rhs=Qff3[:, h, tt * 512:(tt + 1) * 512], start=True, stop=True))
                    for tt in range(2)], reads=[Sin, Qff], writes=[pair])
                yield
                P.op("dve", lambda v: v.tensor_tensor(out=ol[h].ap, in0=ol[h].ap, in1=pair.ap, op=ALU.add), reads=[pair, ol[h]], writes=[ol[h]])
                yield
                P.op("act", lambda a: a.activation(out=sq.ap, in_=ol[h].ap, func=AF.Square), reads=[ol[h]], writes=[sq])
                yield
                pair2 = palloc()
                P.group("pe", [
                    (lambda t, tt=tt: t.matmul(pair2.ap[:, tt * 512:(tt + 1) * 512], lhsT=ones_b, rhs=sq.ap[:, tt * 512:(tt + 1) * 512],
                                               start=True, stop=True)) for tt in range(2)], reads=[sq, CST], writes=[pair2])
                yield
                P.op("act", lambda a: a.activation(out=rs.ap, in_=pair2.ap, func=AF.Ln, bias=epsb, scale=1.0 / 128.0), reads=[pair2, CST], writes=[rs])
                yield
                P.op("act", lambda a: a.activation(out=rs.ap, in_=rs.ap, func=AF.Exp, scale=-0.5), reads=[rs], writes=[rs])
                yield
                s_ = wload("hg", li * 8 + h, 2048)
                wv_ = s_.ap[:, 0:2048].rearrange("p (k m) -> p k m", k=KC)
                pg = palloc()
                mm32(pg, lambda kc: wv_[:, kc, :], [s_])
                yield
                P.op("act", lambda a: a.activation(out=sg.ap, in_=pg.ap, func=AF.Silu), reads=[pg], writes=[sg])
                yield
                P.op("dve", lambda v: v.scalar_tensor_tensor(out=ol[h].ap, in0=ol[h].ap, scalar=pcol("hn", l), in1=rs.ap, op0=ALU.mult, op1=ALU.mult),
                     reads=[ol[h], rs, par], writes=[ol[h]])
                yield
                P.op("dve", lambda v: v.tensor_tensor(out=yh3[:, h, :], in0=ol[h].ap, in1=sg.ap, op=ALU.mult), reads=[ol[h], sg], writes=[yhb])
                yield

            for hp_ in range(4):
                drain(fin(2 * hp_, FS[0]), fin(2 * hp_ + 1, FS[1]))
            cw0 = par_t[:, PO["cw"] + l * 24:PO["cw"] + l * 24 + 8]
            cw1 = par_t[:, PO["cw"] + l * 24 + 8:PO["cw"] + l * 24 + 16]
            up3 = uprev.rearrange("p (j k) -> p j k", k=16)
            cp3 = cpre.rearrange("p (j k) -> p j k", k=16)
            cb3 = cbs.rearrange("p (j k) -> p j k", k=16)
            t13 = t1s.rearrange("p (j k) -> p j k", k=2)
            t23 = t2s.rearrange("p (j k) -> p j k", k=2)
            yf3 = yfix.rearrange("p (j k) -> p j k", k=2)
            w0b = cw0.unsqueeze(2).broadcast_to([128, 8, 2])
            w1b = cw1.unsqueeze(2).broadcast_to([128, 8, 2])
            P.op("dve", lambda v: v.tensor_tensor(out=t13, in0=up3[:, :, 14:16], in1=w0b, op=ALU.mult), reads=[xsm, par], writes=[xsm])
            P.op("dve", lambda v: v.tensor_tensor(out=cp3[:, :, 0:2], in0=cp3[:, :, 0:2], in1=t13, op=ALU.add), reads=[xsm], writes=[xsm])
            P.op("dve", lambda v: v.tensor_tensor(out=t23, in0=up3[:, :, 15:16].broadcast_to([128, 8, 2]), in1=w1b, op=ALU.mult), reads=[xsm, par], writes=[xsm])
            P.op("dve", lambda v: v.tensor_tensor(out=t23, in0=t23, in1=mask10, op=ALU.mult), reads=[xsm, CST], writes=[xsm])
            P.op("dve", lambda v: v.tensor_tensor(out=cp3[:, :, 0:2], in0=cp3[:, :, 0:2], in1=t23, op=ALU.add), reads=[xsm], writes=[xsm])
            P.op("dve", lambda v: v.tensor_tensor(out=yf3, in0=cb3[:, :, 0:2], in1=cp3[:, :, 0:2], op=ALU.mult), reads=[xsm], writes=[xsm])
            P.op("dve", lambda v: v.tensor_copy(out=yc3[:, :, 0:2], in_=yf3), reads=[xsm], writes=[yconv])
            reload_x()
            if MIXCUT == 5:
                return
            RB.reset()
            sgm = RB.get(1024, F32)
            macc = RB.get(1024, F32)
            tmp = RB.get(1024, F32)
            mg = RB.get(4096, BF16)
            mg3 = mg.ap.rearrange("p (k t) -> p k t", k=4)
            Ys = [yc3, yh3, ym3]
            Yb = [yconv, yhb, ymem]
            for grp in range(4):
                for d4 in range(4):
                    dc = grp * 4 + d4
                    for n in range(3):
                        s = wload("mg", (li * 16 + dc) * 3 + n, 3072)
                        gw = s.ap[:, 0:2048].rearrange("p (k m) -> p k m", k=KC)
                        bw = s.ap[:, 2048:3072].rearrange("p (k m) -> p k m", k=8)
                        pG = palloc()
                        mm32(pG, lambda kc, gw=gw: gw[:, kc, :], [s])
                        pP = palloc()
                        P.group("pe", [
                            (lambda t, k8=k8, tt=tt, n=n, pP=pP, bw=bw: t.matmul(pP.ap[:, tt * 512:(tt + 1) * 512], lhsT=bw[:, k8, :],
                                                                                rhs=Ys[n][:, k8, tt * 512:(tt + 1) * 512],
                                                                                start=(k8 == 0), stop=(k8 == 7)))
                            for k8 in range(8) for tt in range(2)], reads=[s, Yb[n]], writes=[pP])
                        P.op("act", lambda a, pG=pG: a.activation(out=sgm.ap, in_=pG.ap, func=AF.Sigmoid), reads=[pG], writes=[sgm])
                        if n == 0:
                            P.op("dve", lambda v, pP=pP: v.tensor_tensor(out=macc.ap, in0=sgm.ap, in1=pP.ap, op=ALU.mult), reads=[sgm, pP], writes=[macc])
                        else:
                            P.op("dve", lambda v, pP=pP: v.tensor_tensor(out=tmp.ap, in0=sgm.ap, in1=pP.ap, op=ALU.mult), reads=[sgm, pP], writes=[tmp])
                            if n == 1:
                                P.op("dve", lambda v: v.tensor_tensor(out=macc.ap, in0=macc.ap, in1=tmp.ap, op=ALU.add), reads=[macc, tmp], writes=[macc])
                            else:
                                P.op("dve", lambda v, d4=d4: v.tensor_tensor(out=mg3[:, d4, :], in0=macc.ap, in1=tmp.ap, op=ALU.add), reads=[macc, tmp], writes=[mg])
                for half in range(2):
                    s = wload("wo", (li * 4 + grp) * 2 + half, 4096)
                    wv = s.ap.rearrange("p (d k m) -> p d k m", d=8, k=4)
                    for d8 in range(8):
                        dcq = half * 8 + d8
                        pair = palloc()
                        P.group("pe", [
                            (lambda t, k4=k4, tt=tt, d8=d8, pair=pair, wv=wv: t.matmul(pair.ap[:, tt * 512:(tt + 1) * 512], lhsT=wv[:, d8, k4, :],
                                                                                      rhs=mg3[:, k4, tt * 512:(tt + 1) * 512],
                                                                                      start=(k4 == 0), stop=(k4 == 3)))
                            for k4 in range(4) for tt in range(2)], reads=[s, mg], writes=[pair])
                        P.op("dve", lambda v, dcq=dcq, pair=pair: v.tensor_tensor(out=xT.ap[:, dcq, :], in0=pair.ap, in1=xT.ap[:, dcq, :], op=ALU.add),
                             reads=[pair, xT], writes=[xT])

        for (kind, l) in stages:
            if kind == "ffn1":
                ffn(l, 1)
            elif kind == "ffn2":
                ffn(l, 2)
            else:
                mixer(l)

        RA.reset()
        P.barrier()
        outb = []
        if final_norm:
            rstd = x_rstd(RA)
            ot = [RA.get(T, F32) for _ in range(2)]
            for c in range(KC):
                o = ot[c % 2]
                P.op("dve", lambda v, c=c, o=o: v.scalar_tensor_tensor(
                    out=o.ap, in0=xT.ap[:, c, :], scalar=pcol("nf", c), in1=rstd.ap,
                    op0=ALU.mult, op1=ALU.mult), reads=[xT, rstd, par], writes=[o])
                tk = P.dma("sp", lambda e, c=c, o=o: e.dma_start(out=y_d[c * 128:(c + 1) * 128, :], in_=o.ap),
                           "st%d" % (c % 2), reads=[o])
                outb.append(tk)
        else:
            for q in range(4):
                tk = P.dma("sp", lambda e, q=q: e.dma_start(
                    out=y_d[q * 512:(q + 1) * 512, :].rearrange("(c p) t -> p c t", p=128),
                    in_=xT.ap[:, q * 4:(q + 1) * 4, :]), "st%d" % q, reads=[xT])
                outb.append(tk)
        P.wait("sp", outb)

        semkeys = ["pe", "act", "dve", "pool", "sp"] + sorted(P.dcnt.keys())
        sems = {}
        for k in semkeys:
            sems[k] = es.enter_context(nc.semaphore("s_" + k))
        with nc.Block() as block:
            def replay(en, h):
                for o in P.E[en].ops:
                    if o[0] == "wait":
                        h.wait_ge(sems[o[1]], o[2])
                    elif o[0] == "op":
                        ins = o[1](h)
                        if o[2]:
                            ins.then_inc(sems[en], 1)
                    elif o[0] == "custom":
                        ins = o[1](h)
                        ins.then_inc(sems[o[2]], o[3])
                    else:
                        ins = o[1](h)
                        ins.then_inc(sems[o[2]], 16)

            @block.tensor
            def _(t):
                replay("pe", t)

            @block.scalar
            def _(a):
                replay("act", a)

            @block.vector
            def _(v):
                replay("dve", v)

            @block.gpsimd
            def _(g):
                replay("pool", g)

            @block.sync
            def _(s):
                replay("sp", s)
    return nc, sorted(W.keys())


def _lhsT(Wl, c0, ncols=128):
    return Wl[:, c0:c0 + ncols].reshape(KC, 128, ncols).transpose(1, 0, 2)


def prep_weights(inp, layers, used):
    out = {}
    NLL = len(layers)
    ls = list(layers)
    for w in (1, 2):
        if ("gu%d" % w) not in used:
            continue
        Wg = inp["ffn%d_w_gate" % w][ls].reshape(NLL, KC, 128, NF, 128)
        Wu = inp["ffn%d_w_up" % w][ls].reshape(NLL, KC, 128, NF, 128)
        gu = np.empty((NLL, NF, 128, 2, KC, 128), np.float32)
        gu[:, :, :, 0] = Wg.transpose(0, 3, 2, 1, 4)
        gu[:, :, :, 1] = Wu.transpose(0, 3, 2, 1, 4)
        out["w_gu%d" % w] = gu.reshape(NLL * NF, 128, 4096)
        Wd = inp["ffn%d_w_down" % w][ls].reshape(NLL, NG, GJ, 128, 8, 2, 128)
        out["w_d%d" % w] = np.ascontiguousarray(Wd.transpose(0, 1, 4, 3, 5, 2, 6)).reshape(NLL * NG * 8, 128, 2816)
    fam = {k: [] for k in ("qf", "vi", "hg", "cxc", "cb", "aq", "mg", "wo", "kk", "kv")}
    for l in (layers if "qf" in used else []):
        Win = inp["w_in"][l]
        Wkv = inp["w_mem_kv"][l]
        Wb = inp["w_branch"][l]
        Wo = inp["w_o"][l]
        for h in range(8):
            fam["qf"].append(np.stack([_lhsT(Win, 3072 + h * 128), _lhsT(Win, 4096 + h * 128)], 1).reshape(128, 4096))
            fam["hg"].append(_lhsT(Win, 6144 + h * 128).reshape(128, 2048))
            fam["cxc"].append(np.stack([_lhsT(Win, h * 128), _lhsT(Win, 2048 + h * 128)], 1).reshape(128, 4096))
            fam["cb"].append(_lhsT(Win, 1024 + h * 128).reshape(128, 2048))
        for q in range(4):
            fam["vi"].append(_lhsT(Win, 5120 + q * 256, 256).reshape(128, 4096))
            fam["aq"].append(np.stack([_lhsT(Win, 7168 + q * 256), _lhsT(Win, 7168 + q * 256 + 128)], 1).reshape(128, 4096))
            fam["kk"].append(np.stack([_lhsT(Wkv, 2 * q * 128), _lhsT(Wkv, (2 * q + 1) * 128)], 1).reshape(128, 4096))
            fam["kv"].append(_lhsT(Wkv, 1024 + q * 256, 256).reshape(128, 4096))
        for dc in range(16):
            for n in range(3):
                g_ = _lhsT(Win, 8192 + n * 2048 + dc * 128).reshape(128, 2048)
                b_ = Wb[n][:, dc * 128:(dc + 1) * 128].reshape(8, 128, 128).transpose(1, 0, 2).reshape(128, 1024)
                fam["mg"].append(np.concatenate([g_, b_], 1))
        wo = Wo.reshape(4, 4, 128, 2, 8, 128).transpose(0, 3, 2, 4, 1, 5).reshape(8, 128, 4096)
        fam["wo"].extend(list(wo))
    for k, v in fam.items():
        if not v:
            continue
        out["w_" + k] = np.ascontiguousarray(np.stack(v, 0), dtype=np.float32)
    return out


def prep_small(inp, core):
    par = np.zeros((128, NPAR), np.float32)

    def put(name, arr):
        par[:, PO[name]:PO[name] + arr.shape[1]] = arr

    for nm, key in (("n1", "norm_ffn1"), ("nm", "norm_mix"), ("n2", "norm_ffn2"), ("nmem", "mem_norm")):
        put(nm, inp[key].reshape(L, KC, 128).transpose(2, 0, 1).reshape(128, L * KC))
    put("nf", inp["final_norm"].reshape(KC, 128).T)
    put("cw", inp["conv_w"].reshape(L, 3, 8, 128).transpose(3, 0, 1, 2).reshape(128, L * 24))
    put("lbl", inp["hgrn_lb_logits"].reshape(L, 8, 128).transpose(2, 0, 1).reshape(128, L * 8))
    put("hn", inp["hgrn_norm"].T)
    par[:, PO["flag"]] = float(core % 2)
    return par


def make_consts():
    c = np.zeros((128, 2048), np.float32)
    c[:, 0:128] = np.eye(128, dtype=np.float32)
    rm = np.ones((128, 1024), np.float32)
    rm[:, 0::64] = 0.0
    c[:, 128:1152] = rm
    s = np.arange(128)[:, None] % 64
    t = np.arange(64)[None, :]
    c[:, 1152:1664] = np.tile((s <= t).astype(np.float32), (1, 8))
    c[:, 1664:1680] = 1.0
    c[:, 1680:1696] = np.tile(np.array([1.0, 0.0], np.float32), 8)
    return c


STAGES_FULL = [(k, l) for l in range(L) for k in ("ffn1", "mix", "ffn2")]
_cache = {}


def run(inp, stages, final_norm=True, trace=False):
    key = (tuple(stages), final_norm)
    if key not in _cache:
        _cache[key] = build(stages, final_norm)
    nc, used = _cache[key]
    x = np.asarray(inp["x"], np.float32)
    mem = np.asarray(inp["mem"], np.float32)
    import time as _t
    _t0 = _t.time()
    layers = sorted({l for _, l in stages})
    wts = prep_weights(inp, layers, used)
    print("[kernel] host prep %.1fs" % (_t.time() - _t0), flush=True)
    cst = make_consts()
    in_maps = []
    for c in range(8):
        b, h = c // 2, c % 2
        m = {"xT": np.ascontiguousarray(x[b, h * T:(h + 1) * T, :].T),
             "memT": np.ascontiguousarray(mem[b].T),
             "par": prep_small(inp, c), "cst": cst}
        m.update({("w_" + k): wts["w_" + k] for k in used})
        in_maps.append(m)
    res = run_bass_kernel_spmd(nc, in_maps, core_ids=list(range(8)), trace=trace)
    out = np.empty((4, 2048, D), np.float32)
    for c in range(8):
        b, h = c // 2, c % 2
        out[b, h * T:(h + 1) * T, :] = res.results[c]["yT"].T
    return out, res


def kernel(**inputs):
    inp = {k: np.asarray(v) for k, v in inputs.items()}
    out, _ = run(inp, STAGES_FULL, True)
    return out
```

```python
import numpy as np
import ml_dtypes
import concourse.bass as bass
import concourse.mybir as mybir
from concourse.bass_utils import run_bass_kernel_spmd

F32 = mybir.dt.float32
BF16 = mybir.dt.bfloat16
AF = mybir.ActivationFunctionType
ALU = mybir.AluOpType

L = 4
D = 2048
T = 1024
KC = 16
FF = 5632
NF = 44
GJ = 11
NG = 4
EPS = 1e-6
MIXCUT = 99
CONVDBG = 0
NSLOT = 3
SLOT = 4096

PO = {}
_o = 0
for _n, _w in [("n1", L * 16), ("nm", L * 16), ("n2", L * 16), ("nmem", L * 16), ("nf", 16),
               ("cw", L * 3 * 8), ("lbl", L * 8), ("hn", L), ("flag", 1)]:
    PO[_n] = _o
    _o += _w
NPAR = _o


class Buf:
    __slots__ = ("w", "r")

    def __init__(self):
        self.w = None
        self.r = []


class TB:
    def __init__(self, ap):
        self.buf = Buf()
        self.ap = ap


class Eng:
    def __init__(self, name):
        self.name = name
        self.ops = []
        self.cnt = 0
        self.seen = {}


class Prog:
    def __init__(self):
        self.E = {n: Eng(n) for n in ["pe", "act", "dve", "pool", "sp"]}
        self.dcnt = {}

    def _deps(self, eng, reads, writes, extra):
        toks = list(extra)
        for b in reads:
            if b.w is not None:
                toks.append(b.w)
        for b in writes:
            if b.w is not None:
                toks.append(b.w)
            toks.extend(b.r)
        for (k, v) in toks:
            if eng.seen.get(k, 0) >= v:
                continue
            eng.seen[k] = v
            eng.ops.append(("wait", k, v))

    def _commit(self, tok, reads, writes):
        for b in reads:
            b.r.append(tok)
        for b in writes:
            b.w = tok
            b.r = []

    def op(self, en, fn, reads=(), writes=(), extra=()):
        return self.group(en, [fn], reads, writes, extra)

    def group(self, en, fns, reads=(), writes=(), extra=()):
        eng = self.E[en]
        reads = [b.buf if isinstance(b, TB) else b for b in reads]
        writes = [b.buf if isinstance(b, TB) else b for b in writes]
        self._deps(eng, reads, writes, extra)
        for fn in fns[:-1]:
            eng.ops.append(("op", fn, False))
        eng.cnt += 1
        tok = (en, eng.cnt)
        eng.ops.append(("op", fns[-1], True))
        self._commit(tok, reads, writes)
        return tok

    def dma(self, en, fn, semkey, reads=(), writes=(), extra=()):
        eng = self.E[en]
        reads = [b.buf if isinstance(b, TB) else b for b in reads]
        writes = [b.buf if isinstance(b, TB) else b for b in writes]
        self._deps(eng, reads, writes, extra)
        self.dcnt[semkey] = self.dcnt.get(semkey, 0) + 16
        tok = (semkey, self.dcnt[semkey])
        eng.ops.append(("dma", fn, semkey))
        self._commit(tok, reads, writes)
        return tok

    def custom(self, en, fn, semkey, inc, reads=(), writes=(), extra=()):
        eng = self.E[en]
        reads = [b.buf if isinstance(b, TB) else b for b in reads]
        writes = [b.buf if isinstance(b, TB) else b for b in writes]
        self._deps(eng, reads, writes, extra)
        self.dcnt[semkey] = self.dcnt.get(semkey, 0) + inc
        tok = (semkey, self.dcnt[semkey])
        eng.ops.append(("custom", fn, semkey, inc))
        self._commit(tok, reads, writes)
        return tok

    def wait(self, en, toks):
        self._deps(self.E[en], [], [], [t for t in toks if t is not None])

    def barrier(self, engs=("pe", "act", "dve"), extra=()):
        toks = [(e, self.E[e].cnt) for e in engs if self.E[e].cnt > 0] + [t for t in extra if t is not None]
        for e in engs:
            if e == "pe":
                continue
            self._deps(self.E[e], [], [], toks)


def build(stages, final_norm=True, ncores=8):
    nc = bass.Bass("TRN2", target_bir_lowering=False)
    P = Prog()
    layers = sorted({l for _, l in stages})
    NL = len(layers)
    LI = {l: i for i, l in enumerate(layers)}

    def din(name, shape, dt=F32):
        return nc.dram_tensor(name, list(shape), dt, kind="ExternalInput").ap()

    x_d = din("xT", [D, T])
    mem_d = din("memT", [D, 256])
    par_d = din("par", [128, NPAR])
    cst_d = din("cst", [128, 2048])
    y_d = nc.dram_tensor("yT", [D, T], F32, kind="ExternalOutput").ap()
    xsp_d = nc.dram_tensor("xspill", [D, T], F32, kind="Internal").ap()
    ccs_d = nc.dram_tensor("cc_src", [128, 1152], F32, kind="Internal").ap()
    ccd_d = nc.dram_tensor("cc_dst", [256, 1152], F32, kind="Internal").ap()
    WSH = {"gu1": [NL * NF, 128, 4096], "gu2": [NL * NF, 128, 4096], "d1": [NL * NG * 8, 128, 2816],
           "d2": [NL * NG * 8, 128, 2816], "qf": [NL * 8, 128, 4096], "vi": [NL * 4, 128, 4096],
           "hg": [NL * 8, 128, 2048], "cxc": [NL * 8, 128, 4096], "cb": [NL * 8, 128, 2048],
           "aq": [NL * 4, 128, 4096], "mg": [NL * 48, 128, 3072], "wo": [NL * 8, 128, 4096],
           "kk": [NL * 4, 128, 4096], "kv": [NL * 4, 128, 4096]}
    W = {}

    def getW(fam):
        if fam not in W:
            W[fam] = din("w_" + fam, WSH[fam])
        return W[fam]

    import contextlib
    es = contextlib.ExitStack()
    with es:
        def sb(name, shape, dt):
            return es.enter_context(nc.sbuf_tensor(name, list(shape), dt))

        hT_t = sb("hT", [128, KC * T], BF16)
        slots_t = sb("slots", [128, NSLOT * SLOT], BF16)
        par_t = sb("par_s", [128, NPAR], F32)
        cstf_t = sb("cstf", [128, 2048], F32)
        cstb_t = sb("cstb", [128, 1152], BF16)
        lb_t = sb("lb", [128, 2 * L * 8 + 16], F32)
        X_t = sb("X", [128, 16384], F32)
        R_t = sb("R", [128, 18432], F32)
        ps_t = es.enter_context(nc.psum_tensor("ps", [128, 4096], F32))

        hT = TB(hT_t[:, :].rearrange("p (c t) -> p c t", c=KC))
        xT = TB(X_t[:, :].rearrange("p (c t) -> p c t", c=KC))
        par = TB(par_t[:, :])
        cstf = cstf_t[:, :]
        ident = cstf[:, 0:128]
        rmask = cstf[:, 128:1152]
        cmask = cstf[:, 1152:1216]
        ones_b = cstb_t[:, 0:128]
        CST = Buf()
        pairs = [TB(ps_t[:, k * 1024:(k + 1) * 1024]) for k in range(4)]
        pstate = {"i": 0}

        reserved = set()
        pend = {}

        def palloc():
            while True:
                k_ = pstate["i"] % 4
                pstate["i"] += 1
                if k_ not in reserved:
                    return pairs[k_]

        slot_bufs = [TB(slots_t[:, k * SLOT:(k + 1) * SLOT]) for k in range(NSLOT)]
        sstate = {"i": 0}

        def wload(fam, idx, n):
            s = slot_bufs[sstate["i"] % NSLOT]
            key = "ws%d" % (sstate["i"] % NSLOT)
            sstate["i"] += 1
            src = getW(fam)[idx]
            dst = s.ap[:, 0:n]
            P.dma("pool", lambda g, dst=dst, src=src: g.dma_start(out=dst, in_=src), key, reads=[], writes=[s])
            return s

        def rbytes_view(tensor, off_f32, n_elems, dt):
            if dt == F32:
                return tensor[:, off_f32:off_f32 + n_elems]
            assert n_elems % 2 == 0
            return tensor[:, off_f32:off_f32 + n_elems // 2].bitcast(BF16)

        class Arena:
            def __init__(self, tensor, base, size):
                self.t = tensor
                self.base = base
                self.size = size
                self.off = 0

            def reset(self):
                self.off = 0

            def get(self, n_elems, dt, init_r=()):
                words = n_elems if dt == F32 else n_elems // 2
                assert self.off + words <= self.size, ("arena overflow", self.off, words, self.size)
                v = rbytes_view(self.t, self.base + self.off, n_elems, dt)
                self.off += words
                tb = TB(v)
                tb.buf.r = list(init_r)
                return tb

        RA = Arena(R_t, 0, 18432)
        RB = Arena(R_t, 12288, 6144)
        RY = [Arena(R_t, 4096 * i, 4096) for i in range(3)]
        XA = Arena(X_t, 0, 16384)

        P.dma("sp", lambda e: e.dma_start(out=par_t[:, :], in_=par_d[:, :]), "ld_par", writes=[par])
        for q in range(4):
            P.dma("sp", lambda e, q=q: e.dma_start(
                out=xT.ap[:, q * 4:(q + 1) * 4, :],
                in_=x_d[q * 512:(q + 1) * 512, :].rearrange("(c p) t -> p c t", p=128)),
                "ld_x%d" % q, writes=[xT])
        P.dma("sp", lambda e: e.dma_start(out=cstf_t[:, :], in_=cst_d[:, :]), "ld_cst", writes=[CST])
        P.op("dve", lambda v: v.memset(cstb_t[:, 0:128], 1.0), writes=[CST])
        lbl = par_t[:, PO["lbl"]:PO["lbl"] + L * 8].rearrange("p (l h) -> p l h", l=L)
        ex_t = lb_t[:, 0:L * 8].rearrange("p (l h) -> p l h", l=L)
        LB = TB(lb_t[:, :])
        oml_v = lb_t[:, L * 8:2 * L * 8].rearrange("p (l h) -> p l h", l=L)
        ssum = lb_t[:, 2 * L * 8:2 * L * 8 + 8]
        P.op("act", lambda a: a.activation(out=ex_t, in_=lbl, func=AF.Exp), reads=[par], writes=[LB])
        P.op("dve", lambda v: v.tensor_tensor(out=ssum, in0=ex_t[:, 0, :], in1=ex_t[:, 1, :], op=ALU.add), reads=[LB], writes=[LB])
        P.op("dve", lambda v: v.tensor_tensor(out=ssum, in0=ssum, in1=ex_t[:, 2, :], op=ALU.add), reads=[LB], writes=[LB])
        P.op("dve", lambda v: v.tensor_tensor(out=ssum, in0=ssum, in1=ex_t[:, 3, :], op=ALU.add), reads=[LB], writes=[LB])
        P.op("dve", lambda v: v.reciprocal(out=ssum, in_=ssum), reads=[LB], writes=[LB])
        for l in range(L):
            P.op("dve", lambda v, l=l: v.tensor_tensor(out=ex_t[:, l, :], in0=ex_t[:, l, :], in1=ssum, op=ALU.mult), reads=[LB], writes=[LB])
        P.op("dve", lambda v: v.tensor_tensor(out=ex_t[:, 3, :], in0=ex_t[:, 3, :], in1=ex_t[:, 2, :], op=ALU.add), reads=[LB], writes=[LB])
        P.op("dve", lambda v: v.tensor_tensor(out=ex_t[:, 3, :], in0=ex_t[:, 3, :], in1=ex_t[:, 1, :], op=ALU.add), reads=[LB], writes=[LB])
        P.op("dve", lambda v: v.tensor_tensor(out=ex_t[:, 2, :], in0=ex_t[:, 2, :], in1=ex_t[:, 1, :], op=ALU.add), reads=[LB], writes=[LB])
        P.op("dve", lambda v: v.memset(ex_t[:, 0, :], 0.0), reads=[LB], writes=[LB])
        P.op("dve", lambda v: v.tensor_scalar(out=oml_v, in0=ex_t, scalar1=-1.0, scalar2=1.0, op0=ALU.mult, op1=ALU.add), reads=[LB], writes=[LB])
        lb_v = ex_t

        def pcol(name, i):
            return par_t[:, PO[name] + i:PO[name] + i + 1]

        def rms_stats(src_ap_fn, src_buf, nchunks, ntok, inv_n, arena):
            pair = palloc()
            sq = [arena.get(ntok, BF16) for _ in range(2)]
            nt = (ntok + 511) // 512
            w = min(ntok, 512)
            for c in range(nchunks):
                s = sq[c % 2]
                if c % 2 == 0:
                    P.op("act", lambda a, c=c, s=s: a.activation(out=s.ap, in_=src_ap_fn(c), func=AF.Square),
                         reads=[src_buf], writes=[s])
                else:
                    P.op("dve", lambda v, c=c, s=s: v.tensor_tensor(out=s.ap, in0=src_ap_fn(c), in1=src_ap_fn(c), op=ALU.mult),
                         reads=[src_buf], writes=[s])
                P.group("pe", [
                    (lambda t, c=c, s=s, tt=tt: t.matmul(pair.ap[:, tt * 512:tt * 512 + w], lhsT=ones_b,
                                                        rhs=s.ap[:, tt * w:(tt + 1) * w],
                                                        start=(c == 0), stop=(c == nchunks - 1)))
                    for tt in range(nt)], reads=[s, CST], writes=[pair])
            rstd = arena.get(ntok, F32)
            if nt == 2:
                pv = pair.ap[:, 0:ntok]
            else:
                pv = pair.ap[:, 0:w]
            P.op("act", lambda a: a.activation(out=rstd.ap, in_=pv, func=AF.Ln, bias=epsb, scale=inv_n),
                 reads=[pair, CST], writes=[rstd])
            P.op("act", lambda a: a.activation(out=rstd.ap, in_=rstd.ap, func=AF.Exp, scale=-0.5),
                 reads=[rstd], writes=[rstd])
            return rstd

        epsb = lb_t[:, 2 * L * 8 + 8:2 * L * 8 + 9]
        P.op("dve", lambda v: v.memset(epsb, EPS), writes=[CST])

        class FusedStats:
            def __init__(self, arena):
                self.pair = palloc()
                self.k = pairs.index(self.pair)
                reserved.add(self.k)
                self.sq = [arena.get(T, BF16) for _ in range(4)]
                self.q = []
                self.n = 0

            def _mm(self, i, s_):
                pair = self.pair
                P.group("pe", [
                    (lambda t, tt=tt: t.matmul(pair.ap[:, tt * 512:(tt + 1) * 512], lhsT=ones_b,
                                               rhs=s_.ap[:, tt * 512:(tt + 1) * 512], start=(i == 0), stop=(i == KC - 1)))
                    for tt in range(2)], reads=[s_, CST], writes=[pair])

            def chunk(self, c):
                s_ = self.sq[self.n % 4]
                P.op("act", lambda a: a.activation(out=s_.ap, in_=xT.ap[:, c, :], func=AF.Square), reads=[xT], writes=[s_])
                self.q.append((self.n, s_))
                self.n += 1
                if len(self.q) > 2:
                    self._mm(*self.q.pop(0))

            def flush(self):
                while self.q:
                    self._mm(*self.q.pop(0))

        def x_rstd(arena):
            fs = pend.pop("fs", None)
            if fs is None:
                return rms_stats(lambda c: xT.ap[:, c, :], xT, KC, T, 1.0 / D, arena)
            assert fs.n == KC
            rstd = arena.get(T, F32)
            pair = fs.pair
            P.op("act", lambda a: a.activation(out=rstd.ap, in_=pair.ap, func=AF.Ln, bias=epsb, scale=1.0 / D),
                 reads=[pair, CST], writes=[rstd])
            P.op("act", lambda a: a.activation(out=rstd.ap, in_=rstd.ap, func=AF.Exp, scale=-0.5),
                 reads=[rstd], writes=[rstd])
            reserved.discard(fs.k)
            return rstd

        def norm_to_hT(gname, l, arena):
            rstd = x_rstd(arena)
            for c in range(KC):
                P.op("dve", lambda v, c=c: v.scalar_tensor_tensor(
                    out=hT.ap[:, c, :], in0=xT.ap[:, c, :], scalar=pcol(gname, l * 16 + c), in1=rstd.ap,
                    op0=ALU.mult, op1=ALU.mult), reads=[xT, rstd, par], writes=[hT])

        def ffn(l, which):
            RA.reset()
            P.barrier()
            norm_to_hT("n1" if which == 1 else "n2", l, RA)
            hid = RA.get(GJ * T, BF16)
            hv = hid.ap.rearrange("p (j t) -> p j t", j=GJ)
            sg = [RA.get(T, F32) for _ in range(2)]
            gu = "gu%d" % which
            dn = "d%d" % which
            for g in range(NG):
                for j in range(GJ):
                    f = g * GJ + j
                    s = wload(gu, LI[l] * NF + f, 4096)
                    wv = s.ap.rearrange("p (g k m) -> p g k m", g=2, k=KC)
                    pp = []
                    for q in range(2):
                        pair = palloc()
                        pp.append(pair)
                        P.group("pe", [
                            (lambda t, q=q, kc=kc, tt=tt, pair=pair, wv=wv: t.matmul(
                                pair.ap[:, tt * 512:(tt + 1) * 512], lhsT=wv[:, q, kc, :],
                                rhs=hT.ap[:, kc, tt * 512:(tt + 1) * 512], start=(kc == 0), stop=(kc == KC - 1)))
                            for kc in range(KC) for tt in range(2)], reads=[s, hT], writes=[pair])
                    st = sg[f % 2]
                    P.op("act", lambda a, st=st, pg=pp[0]: a.activation(out=st.ap, in_=pg.ap, func=AF.Silu),
                         reads=[pp[0]], writes=[st])
                    P.op("dve", lambda v, st=st, pu=pp[1], j=j: v.tensor_tensor(
                        out=hv[:, j, :], in0=st.ap, in1=pu.ap, op=ALU.mult), reads=[st, pp[1]], writes=[hid])
                fs = FusedStats(RA) if g == NG - 1 else None
                for dcp in range(8):
                    s = wload(dn, (LI[l] * NG + g) * 8 + dcp, 2816)
                    wv = s.ap[:, 0:2816].rearrange("p (d j m) -> p d j m", d=2, j=GJ)
                    for d2 in range(2):
                        dc = dcp * 2 + d2
                        pair = palloc()
                        P.group("pe", [
                            (lambda t, d2=d2, j=j, tt=tt, pair=pair, wv=wv: t.matmul(
                                pair.ap[:, tt * 512:(tt + 1) * 512], lhsT=wv[:, d2, j, :],
                                rhs=hv[:, j, tt * 512:(tt + 1) * 512], start=(j == 0), stop=(j == GJ - 1)))
                            for j in range(GJ) for tt in range(2)], reads=[s, hid], writes=[pair])
                        P.op("dve", lambda v, dc=dc, pair=pair: v.scalar_tensor_tensor(
                            out=xT.ap[:, dc, :], in0=pair.ap, scalar=0.5, in1=xT.ap[:, dc, :],
                            op0=ALU.mult, op1=ALU.add), reads=[pair, xT], writes=[xT])
                        if fs is not None:
                            fs.chunk(dc)
                if fs is not None:
                    fs.flush()
                    pend["fs"] = fs

        cmask8 = cstf[:, 1152:1664].rearrange("p (b t) -> p b t", b=8)
        ones16 = cstf[:, 1664:1680]
        rg_pairs = [[2 * i, 2 * i + 1] for i in range(ncores // 2)]

        def mm32(pair, lhs_fn, reads):
            return P.group("pe", [
                (lambda t, kc=kc, tt=tt: t.matmul(pair.ap[:, tt * 512:(tt + 1) * 512], lhsT=lhs_fn(kc),
                                                  rhs=hT.ap[:, kc, tt * 512:(tt + 1) * 512],
                                                  start=(kc == 0), stop=(kc == KC - 1)))
                for kc in range(KC) for tt in range(2)], reads=list(reads) + [hT], writes=[pair])

        def mixer(l):
            li = LI[l]
            RA.reset()
            P.barrier()
            norm_to_hT("nm", l, RA)
            spill = []
            for q in range(4):
                spill.append(P.dma("sp", lambda e, q=q: e.dma_start(
                    out=xsp_d[q * 512:(q + 1) * 512, :].rearrange("(c p) t -> p c t", p=128),
                    in_=xT.ap[:, q * 4:(q + 1) * 4, :]), "sp_x%d" % q, reads=[xT]))
            P.barrier()

            def reload_x():
                P.barrier()
                bt = [(e, P.E[e].cnt) for e in ("pe", "act", "dve")]
                for q in range(4):
                    P.dma("sp", lambda e, q=q: e.dma_start(
                        out=xT.ap[:, q * 4:(q + 1) * 4, :],
                        in_=xsp_d[q * 512:(q + 1) * 512, :].rearrange("(c p) t -> p c t", p=128)),
                        "ld_x%d" % q, writes=[xT], extra=bt + spill)
            if MIXCUT == 0:
                reload_x()
                return
            RA.reset()
            XA.reset()
            Vt = RA.get(8192, BF16)
            Vv = Vt.ap.rearrange("p (b c) -> p b c", b=8)
            A, B, C, Dd, E, Q = [RA.get(1024, F32) for _ in range(6)]
            Qt, Kb, Qh = [RA.get(1024, BF16) for _ in range(3)]
            Kp = RA.get(2048, BF16)
            Kp4 = Kp.ap.rearrange("p (b two d) -> p b two d", two=2, d=128)
            Kp3 = Kp.ap.rearrange("p (c d) -> p c d", d=128)
            Sm = RA.get(1024, BF16)
            Sm4 = Sm.ap.rearrange("p (b two t) -> p b two t", two=2, t=64)
            Sm3 = Sm.ap.rearrange("p (c t) -> p c t", t=64)
            Sall = RA.get(2048, F32)
            Sall3 = Sall.ap.rearrange("p (c d) -> p c d", d=128)
            Sb = RA.get(2048, BF16)
            Sb3 = Sb.ap.rearrange("p (c d) -> p c d", d=128)
            sm = RA.get(128, F32)
            dLm, eLm, aC, BcI, Bx = [sm.ap[:, i * 16:(i + 1) * 16] for i in range(5)]
            Qff = XA.get(8192, BF16, spill)
            Qff3 = Qff.ap.rearrange("p (h t) -> p h t", h=8)
            oloc = XA.get(8192, F32, spill)
            oloc3 = oloc.ap.rearrange("p (h t) -> p h t", h=8)
            sendb = XA.get(1152, F32, spill)
            recvb = XA.get(1152, F32, spill)
            Sin = XA.get(1024, BF16, spill)
            Sin3 = Sin.ap.rearrange("p (h d) -> p h d", h=8)
            xsm = XA.get(640, F32, spill)
            uprev = xsm.ap[:, 0:128]
            cpre = xsm.ap[:, 128:256]
            cbs = xsm.ap[:, 256:384]
            t1s = xsm.ap[:, 384:400]
            t2s = xsm.ap[:, 400:416]
            yfix = xsm.ap[:, 416:432]
            mask10 = cstf[:, 1680:1696].rearrange("p (j k) -> p j k", k=2)
            P.op("dve", lambda v: v.memset(Kp.ap, 0.0), writes=[Kp])
            P.op("dve", lambda v: v.memset(Sm.ap, 0.0), writes=[Sm])
            for hp in range(4):
                s = wload("vi", li * 4 + hp, 4096)
                wv = s.ap.rearrange("p (k n) -> p k n", k=KC)
                for tbp in range(2):
                    pair = palloc()
                    P.group("pe", [
                        (lambda t, tbi=tbi, kc=kc, pair=pair, wv=wv, tbp=tbp: t.matmul(
                            pair.ap[:, tbi * 256:(tbi + 1) * 256],
                            lhsT=hT.ap[:, kc, (tbp * 4 + tbi) * 128:(tbp * 4 + tbi + 1) * 128],
                            rhs=wv[:, kc, :], start=(kc == 0), stop=(kc == KC - 1)))
                        for tbi in range(4) for kc in range(KC)], reads=[s, hT], writes=[pair])
                    P.op("act", lambda a, pair=pair, tbp=tbp, hp=hp: a.activation(
                        out=Vv[:, tbp * 4:(tbp + 1) * 4, hp * 256:(hp + 1) * 256],
                        in_=pair.ap.rearrange("p (b n) -> p b n", b=4), func=AF.Copy), reads=[pair], writes=[Vt])
            def halves(tb):
                return [TB(tb.ap[:, 0:512]), TB(tb.ap[:, 512:1024])]

            def v3(tb):
                return tb.ap.rearrange("p (c t) -> p c t", t=64)

            Ah, Bh, Ch, Dh, Eh, Qs, Qth, Kbh, Qhh = [halves(x_) for x_ in (A, B, C, Dd, E, Q, Qt, Kb, Qh)]
            C3 = C.ap.rearrange("p (c t) -> p c t", t=64)
            A3 = A.ap.rearrange("p (c t) -> p c t", t=64)
            B3 = B.ap.rearrange("p (c t) -> p c t", t=64)
            D3 = Dd.ap.rearrange("p (c t) -> p c t", t=64)
            tstate = {"i": 0}

            def talloc():
                p_ = pairs[2 + tstate["i"] % 2]
                tstate["i"] += 1
                return p_

            Qt_b, Kb_b, Qh_b = [RA.get(1024, BF16) for _ in range(3)]
            sm_b = RA.get(128, F32)
            Kp_b = TB(recvb.ap[:, 0:1024].bitcast(BF16))
            Kp_b.buf.r = list(spill)
            P.op("dve", lambda v: v.memset(Kp_b.ap, 0.0), writes=[Kp_b])
            PAR = []
            for (qt_, kb_, qh_, kp_, sm_) in ((Qt, Kb, Qh, Kp, sm), (Qt_b, Kb_b, Qh_b, Kp_b, sm_b)):
                d_ = {"Qth": halves(qt_), "Kbh": halves(kb_), "Qhh": halves(qh_), "Kp": kp_,
                      "Kp4": kp_.ap.rearrange("p (b two d) -> p b two d", two=2, d=128),
                      "Kp3": kp_.ap.rearrange("p (c d) -> p c d", d=128),
                      "Qt": qt_, "Kb": kb_, "Qh": qh_, "sm": sm_}
                for i_, nm_ in enumerate(("dLm", "eLm", "aC", "BcI", "Bx")):
                    d_[nm_] = sm_.ap[:, i_ * 16:(i_ + 1) * 16]
                PAR.append(d_)

            def emit_proj(h_):
                s_ = wload("qf", li * 8 + h_, 4096)
                wv_ = s_.ap.rearrange("p (g k m) -> p g k m", g=2, k=KC)
                mm32(pairs[0], lambda kc, wv_=wv_: wv_[:, 0, kc, :], [s_])
                mm32(pairs[1], lambda kc, wv_=wv_: wv_[:, 1, kc, :], [s_])

            def part1(h):
                pr = PAR[h % 2]
                Qth, Kbh, Qhh = pr["Qth"], pr["Kbh"], pr["Qhh"]
                Kp4_, smb = pr["Kp4"], pr["sm"]
                dLm, eLm, aC, BcI, Bx = pr["dLm"], pr["eLm"], pr["aC"], pr["BcI"], pr["Bx"]
                pq = pairs[0]
                pz = pairs[1]
                lbc = lb_v[:, l, h:h + 1]
                omlc = oml_v[:, l, h:h + 1]
                H2 = (0, 1)
                for tt in H2:
                    P.op("act", lambda a, tt=tt: a.activation(out=Ah[tt].ap, in_=pz.ap[:, tt * 512:(tt + 1) * 512], func=AF.Sigmoid), reads=[pz], writes=[Ah[tt]])
                    yield
                for tt in H2:
                    P.op("act", lambda a, tt=tt: a.activation(out=Bh[tt].ap, in_=pz.ap[:, tt * 512:(tt + 1) * 512], func=AF.Sigmoid, scale=-1.0), reads=[pz], writes=[Bh[tt]])
                    yield
                for tt in H2:
                    P.op("act", lambda a, tt=tt: a.activation(out=Qs[tt].ap, in_=pq.ap[:, tt * 512:(tt + 1) * 512], func=AF.Copy), reads=[pq], writes=[Qs[tt]])
                    yield
                if h + 1 < 8:
                    emit_proj(h + 1)
                    yield
                for tt in H2:
                    P.op("dve", lambda v, tt=tt: v.tensor_scalar(out=Ah[tt].ap, in0=Ah[tt].ap, scalar1=omlc, scalar2=lbc, op0=ALU.mult, op1=ALU.add), reads=[Ah[tt], LB], writes=[Ah[tt]])
                    yield
                for tt in H2:
                    P.op("dve", lambda v, tt=tt: v.tensor_scalar(out=Ah[tt].ap, in0=Ah[tt].ap, scalar1=1e-30, scalar2=None, op0=ALU.max), reads=[Ah[tt]], writes=[Ah[tt]])
                    yield
                for tt in H2:
                    P.op("act", lambda a, tt=tt: a.activation(out=Ah[tt].ap, in_=Ah[tt].ap, func=AF.Ln), reads=[Ah[tt]], writes=[Ah[tt]])
                    yield
                for tt in H2:
                    P.op("dve", lambda v, tt=tt: v.tensor_tensor_scan(out=Ch[tt].ap, data0=rmask[:, tt * 512:(tt + 1) * 512], data1=Ah[tt].ap, initial=0.0, op0=ALU.mult, op1=ALU.add), reads=[Ah[tt], CST], writes=[Ch[tt]])
                    yield
                for tt in H2:
                    P.op("dve", lambda v, tt=tt: v.tensor_tensor(out=v3(Dh[tt]), in0=v3(Ch[tt]), in1=v3(Ch[tt])[:, :, 31:32].broadcast_to([128, 8, 64]), op=ALU.subtract), reads=[Ch[tt]], writes=[Dh[tt]])
                    yield
                for tt in H2:
                    P.op("act", lambda a, tt=tt: a.activation(out=Eh[tt].ap, in_=Dh[tt].ap, func=AF.Exp), reads=[Dh[tt]], writes=[Eh[tt]])
                    yield
                for tt in H2:
                    P.op("dve", lambda v, tt=tt: v.tensor_tensor(out=Qth[tt].ap, in0=Qs[tt].ap, in1=Eh[tt].ap, op=ALU.mult), reads=[Qs[tt], Eh[tt]], writes=[Qth[tt]])
                    yield
                for tt in H2:
                    P.op("act", lambda a, tt=tt: a.activation(out=Eh[tt].ap, in_=Dh[tt].ap, func=AF.Exp, scale=-1.0), reads=[Dh[tt]], writes=[Eh[tt]])
                    yield
                for tt in H2:
                    P.op("dve", lambda v, tt=tt: v.scalar_tensor_tensor(out=Bh[tt].ap, in0=Bh[tt].ap, scalar=omlc, in1=Eh[tt].ap, op0=ALU.mult, op1=ALU.mult), reads=[Bh[tt], Eh[tt], LB], writes=[Bh[tt]])
                    yield
                for tt in H2:
                    P.op("act", lambda a, tt=tt: a.activation(out=Kbh[tt].ap, in_=Bh[tt].ap, func=AF.Copy), reads=[Bh[tt]], writes=[Kbh[tt]])
                    yield
                P.op("dve", lambda v: v.tensor_tensor(out=dLm, in0=C3[:, :, 63], in1=C3[:, :, 31], op=ALU.subtract), reads=Ch, writes=[smb])
                yield
                P.op("act", lambda a: a.activation(out=eLm, in_=dLm, func=AF.Exp), reads=[smb], writes=[smb])
                yield
                P.op("act", lambda a: a.activation(out=aC, in_=C3[:, :, 63], func=AF.Exp), reads=Ch + [smb], writes=[smb])
                yield
                for tt in H2:
                    P.op("dve", lambda v, tt=tt: v.tensor_tensor(out=v3(Dh[tt]), in0=v3(Bh[tt]), in1=eLm[:, tt * 8:(tt + 1) * 8].unsqueeze(2).broadcast_to([128, 8, 64]), op=ALU.mult), reads=[Bh[tt], smb], writes=[Dh[tt]])
                    yield
                ptr = talloc()
                P.group("pe", [
                    (lambda t, tb=tb: t.transpose(out=ptr.ap[:, tb * 128:(tb + 1) * 128], in_=Dd.ap[:, tb * 128:(tb + 1) * 128], identity=ident))
                    for tb in range(8)], reads=Dh + [CST], writes=[ptr])
                yield
                ptr3 = ptr.ap.rearrange("p (b d) -> p b d", d=128)
                P.op("act", lambda a: a.activation(out=Kp4_[0:64, :, 0, :], in_=ptr3[0:64, :, :], func=AF.Copy), reads=[ptr], writes=[pr["Kp"]])
                yield
                P.op("act", lambda a: a.activation(out=Kp4_[64:128, :, 1, :], in_=ptr3[64:128, :, :], func=AF.Copy), reads=[ptr], writes=[pr["Kp"]])
                yield
                for tt in H2:
                    P.op("act", lambda a, tt=tt: a.activation(out=Eh[tt].ap, in_=Ch[tt].ap, func=AF.Exp), reads=[Ch[tt]], writes=[Eh[tt]])
                    yield
                for tt in H2:
                    P.op("dve", lambda v, tt=tt: v.tensor_tensor(out=Qhh[tt].ap, in0=Qs[tt].ap, in1=Eh[tt].ap, op=ALU.mult), reads=[Qs[tt], Eh[tt]], writes=[Qhh[tt]])
                    yield
                P.op("dve", lambda v: v.tensor_tensor_scan(out=BcI, data0=ones16, data1=C3[:, :, 63], initial=0.0, op0=ALU.mult, op1=ALU.add), reads=Ch + [CST, smb], writes=[smb])
                yield
                P.op("dve", lambda v: v.tensor_tensor(out=Bx, in0=BcI, in1=C3[:, :, 63], op=ALU.subtract), reads=Ch + [smb], writes=[smb])
                yield
                for tt in H2:
                    P.op("dve", lambda v, tt=tt: v.tensor_tensor(out=v3(Ah[tt]), in0=v3(Ch[tt]), in1=Bx[:, tt * 8:(tt + 1) * 8].unsqueeze(2).broadcast_to([128, 8, 64]), op=ALU.add), reads=[Ch[tt], smb], writes=[Ah[tt]])
                    yield
                for tt in H2:
                    P.op("act", lambda a, tt=tt: a.activation(out=Eh[tt].ap, in_=Ah[tt].ap, func=AF.Exp), reads=[Ah[tt]], writes=[Eh[tt]])
                    yield
                for tt in H2:
                    P.op("dve", lambda v, tt=tt: v.tensor_tensor(out=Qff3[:, h, tt * 512:(tt + 1) * 512], in0=Qs[tt].ap, in1=Eh[tt].ap, op=ALU.mult), reads=[Qs[tt], Eh[tt]], writes=[Qff])
                    yield

            def tail(h):
                pr = PAR[h % 2]
                Qth, Kbh, Qhh = pr["Qth"], pr["Kbh"], pr["Qhh"]
                Kp3_, smb, aC = pr["Kp3"], pr["sm"], pr["aC"]
                Kb_, Qt_, Qh_ = pr["Kb"], pr["Qt"], pr["Qh"]
                pU = [talloc(), talloc()]
                P.group("pe", [
                    (lambda t, c=c: t.matmul(pU[c // 8].ap[:, (c % 8) * 128:(c % 8 + 1) * 128], lhsT=Kp3_[:, c, :],
                                             rhs=Vv[:, c // 2, h * 128:(h + 1) * 128], start=True, stop=True))
                    for c in range(16)], reads=[pr["Kp"], Vt], writes=pU)
                yield
                P.op("dve", lambda v: v.tensor_copy(out=Sall3[:, 0, :], in_=pU[0].ap[:, 0:128]), reads=[pU[0]], writes=[Sall])
                yield
                for c in range(1, 16):
                    P.op("dve", lambda v, c=c: v.scalar_tensor_tensor(
                        out=Sall3[:, c, :], in0=Sall3[:, c - 1, :], scalar=aC[:, c:c + 1],
                        in1=pU[c // 8].ap[:, (c % 8) * 128:(c % 8 + 1) * 128], op0=ALU.mult, op1=ALU.add),
                        reads=[Sall, smb, pU[c // 8]], writes=[Sall])
                    yield
                P.op("act", lambda a: a.activation(out=Sb.ap, in_=Sall.ap, func=AF.Copy), reads=[Sall], writes=[Sb])
                yield
                P.op("act", lambda a: a.activation(out=sendb.ap[:, h * 128:(h + 1) * 128], in_=Sall3[:, 15, :], func=AF.Copy), reads=[Sall], writes=[sendb])
                yield
                pS = talloc()
                P.group("pe", [
                    (lambda t, c=c: t.matmul(pS.ap[(c % 2) * 64:(c % 2) * 64 + 64, (c // 2) * 64:(c // 2) * 64 + 64],
                                             lhsT=Kb_.ap[:, c * 64:(c + 1) * 64], rhs=Qt_.ap[:, c * 64:(c + 1) * 64],
                                             start=True, stop=True))
                    for c in range(16)], reads=Kbh + Qth, writes=[pS])
                yield
                pS3 = pS.ap[:, 0:512].rearrange("p (b t) -> p b t", t=64)
                P.op("dve", lambda v: v.tensor_tensor(out=Sm4[0:64, :, 0, :], in0=pS3[0:64, :, :], in1=cmask8[0:64, :, :], op=ALU.mult), reads=[pS, CST], writes=[Sm])
                yield
                P.op("dve", lambda v: v.tensor_tensor(out=Sm4[64:128, :, 1, :], in0=pS3[64:128, :, :], in1=cmask8[64:128, :, :], op=ALU.mult), reads=[pS, CST], writes=[Sm])
                yield
                pO = talloc()
                fns = []
                for c in range(16):
                    fns.append(lambda t, c=c: t.matmul(pO.ap[:, c * 64:(c + 1) * 64], lhsT=Vv[:, c // 2, h * 128:(h + 1) * 128],
                                                       rhs=Sm3[:, c, :], start=True, stop=(c == 0)))
                    if c > 0:
                        fns.append(lambda t, c=c: t.matmul(pO.ap[:, c * 64:(c + 1) * 64], lhsT=Sb3[:, c - 1, :],
                                                           rhs=Qh_.ap[:, c * 64:(c + 1) * 64], start=False, stop=True))
                P.group("pe", fns, reads=[Vt, Sm, Sb] + Qhh, writes=[pO])
                yield
                P.op("act", lambda a: a.activation(out=oloc3[:, h, :], in_=pO.ap, func=AF.Copy), reads=[pO], writes=[oloc])
                yield

            def drain(*gens):
                gens = list(gens)
                while gens:
                    for g_ in list(gens):
                        try:
                            next(g_)
                        except StopIteration:
                            gens.remove(g_)

            emit_proj(0)
            drain(part1(0))
            for h in range(8):
                if h + 1 < 8:
                    drain(part1(h + 1), tail(h))
                else:
                    drain(tail(h))
            if MIXCUT == 1:
                reload_x()
                return
            P.barrier()
            RB.reset()
            for a_ in RY:
                a_.reset()
            yconv = RY[0].get(8192, BF16)
            yh = RY[1]
            ymem_a = RY[2]
            yc3 = yconv.ap.rearrange("p (j t) -> p j t", j=8)
            xs = RB.get(1024, F32)
            ub = RB.get(1040, F32)
            acc = RB.get(1024, F32)
            P.op("dve", lambda v: v.memset(ub.ap[:, 0:16], 0.0), writes=[ub])
            for j in range(8):
                s1 = wload("cxc", li * 8 + j, 4096)
                wv1 = s1.ap.rearrange("p (g k m) -> p g k m", g=2, k=KC)
                s2 = wload("cb", li * 8 + j, 2048)
                wv2 = s2.ap[:, 0:2048].rearrange("p (k m) -> p k m", k=KC)
                pX = palloc()
                mm32(pX, lambda kc, wv1=wv1: wv1[:, 0, kc, :], [s1])
                pC = palloc()
                mm32(pC, lambda kc, wv1=wv1: wv1[:, 1, kc, :], [s1])
                pB = palloc()
                mm32(pB, lambda kc, wv2=wv2: wv2[:, kc, :], [s2])
                w0 = pcol("cw", l * 24 + 0 * 8 + j)
                w1 = pcol("cw", l * 24 + 1 * 8 + j)
                w2 = pcol("cw", l * 24 + 2 * 8 + j)
                P.op("act", lambda a, pX=pX: a.activation(out=xs.ap, in_=pX.ap, func=AF.Copy), reads=[pX], writes=[xs])
                P.op("dve", lambda v, pC=pC: v.tensor_tensor(out=ub.ap[:, 16:1040], in0=pC.ap, in1=xs.ap, op=ALU.mult), reads=[pC, xs], writes=[ub])
                if CONVDBG >= 2:
                    P.op("dve", lambda v, j=j, pB=pB: v.tensor_tensor(out=yc3[:, j, :], in0=pB.ap, in1=ub.ap[:, 16:1040], op=ALU.mult), reads=[pB, ub], writes=[yconv])
                    continue
                P.op("act", lambda a, w1=w1: a.activation(out=xs.ap, in_=ub.ap[:, 15:1039], func=AF.Identity, scale=w1), reads=[ub, par], writes=[xs])
                P.op("dve", lambda v, w2=w2: v.scalar_tensor_tensor(out=acc.ap, in0=ub.ap[:, 16:1040], scalar=w2, in1=xs.ap, op0=ALU.mult, op1=ALU.add), reads=[ub, par, xs], writes=[acc])
                P.op("dve", lambda v, w0=w0: v.scalar_tensor_tensor(out=acc.ap, in0=ub.ap[:, 14:1038], scalar=w0, in1=acc.ap, op0=ALU.mult, op1=ALU.add), reads=[ub, par, acc], writes=[acc])
                P.op("dve", lambda v, j=j, pB=pB: v.tensor_tensor(out=yc3[:, j, :], in0=pB.ap, in1=acc.ap, op=ALU.mult), reads=[pB, acc], writes=[yconv])
                if CONVDBG >= 1:
                    continue
                P.op("dve", lambda v, j=j: v.tensor_copy(out=sendb.ap[:, 1024 + 16 * j:1040 + 16 * j], in_=ub.ap[:, 1024:1040]), reads=[ub], writes=[sendb])
                P.op("dve", lambda v, j=j: v.tensor_copy(out=cpre[:, 16 * j:16 * j + 16], in_=acc.ap[:, 0:16]), reads=[acc], writes=[xsm])
                P.op("dve", lambda v, j=j, pB=pB: v.tensor_copy(out=cbs[:, 16 * j:16 * j + 16], in_=pB.ap[:, 0:16]), reads=[pB], writes=[xsm])
            if MIXCUT == 2:
                reload_x()
                return
            t_s = P.dma("pool", lambda g: g.dma_start(out=ccs_d[:, :], in_=sendb.ap), "cc_s", reads=[sendb])
            t_c = P.custom("pool", lambda g: g.collective_compute("AllGather", ALU.bypass, replica_groups=rg_pairs,
                                                                  ins=[ccs_d[:, :]], outs=[ccd_d[:, :]]), "cc_c", 1, extra=[t_s])
            P.dma("pool", lambda g: g.dma_start(out=recvb.ap, in_=ccd_d[0:128, :]), "cc_r", writes=[recvb], extra=[t_c])
            if MIXCUT == 3:
                P.wait("dve", [P.dcnt and ("cc_r", P.dcnt["cc_r"])])
                reload_x()
                return
            P.barrier()
            RB.reset()
            RY[1].reset()
            memraw = RB.get(4096, F32)
            mr3 = memraw.ap.rearrange("p (c m) -> p c m", c=KC)
            memn = RB.get(4096, BF16)
            mn3 = memn.ap.rearrange("p (c m) -> p c m", c=KC)
            kT = RY[1].get(2048, BF16)
            kT3 = kT.ap.rearrange("p (j m) -> p j m", j=8)
            vM = RY[1].get(2048, BF16)
            vM3 = vM.ap.rearrange("p (b n) -> p b n", b=2)
            bt3 = [(e, P.E[e].cnt) for e in ("pe", "act", "dve")]
            for q in range(4):
                P.dma("sp", lambda e, q=q: e.dma_start(out=mr3[:, q * 4:(q + 1) * 4, :],
                                                       in_=mem_d[q * 512:(q + 1) * 512, :].rearrange("(c p) m -> p c m", p=128)),
                      "ld_mem%d" % q, writes=[memraw], extra=bt3)
            rstd = rms_stats(lambda c: mr3[:, c, :], memraw, KC, 256, 1.0 / D, RY[1])
            for c in range(KC):
                P.op("dve", lambda v, c=c: v.scalar_tensor_tensor(out=mn3[:, c, :], in0=mr3[:, c, :], scalar=pcol("nmem", l * 16 + c),
                                                                  in1=rstd.ap, op0=ALU.mult, op1=ALU.mult), reads=[memraw, rstd, par], writes=[memn])
            for jp in range(4):
                s = wload("kk", li * 4 + jp, 4096)
                wv = s.ap.rearrange("p (g k m) -> p g k m", g=2, k=KC)
                pair = palloc()
                P.group("pe", [
                    (lambda t, j2=j2, kc=kc, pair=pair, wv=wv: t.matmul(pair.ap[:, j2 * 256:(j2 + 1) * 256], lhsT=wv[:, j2, kc, :],
                                                                       rhs=mn3[:, kc, :], start=(kc == 0), stop=(kc == KC - 1)))
                    for j2 in range(2) for kc in range(KC)], reads=[s, memn], writes=[pair])
                P.op("act", lambda a, jp=jp, pair=pair: a.activation(out=kT3[:, jp * 2:(jp + 1) * 2, :],
                                                                     in_=pair.ap[:, 0:512].rearrange("p (j m) -> p j m", j=2), func=AF.Copy),
                     reads=[pair], writes=[kT])
            for vq in range(4):
                s = wload("kv", li * 4 + vq, 4096)
                wv = s.ap.rearrange("p (k n) -> p k n", k=KC)
                pair = palloc()
                P.group("pe", [
                    (lambda t, mb=mb, kc=kc, pair=pair, wv=wv: t.matmul(pair.ap[:, mb * 256:(mb + 1) * 256],
                                                                       lhsT=mn3[:, kc, mb * 128:(mb + 1) * 128],
                                                                       rhs=wv[:, kc, :], start=(kc == 0), stop=(kc == KC - 1)))
                    for mb in range(2) for kc in range(KC)], reads=[s, memn], writes=[pair])
                P.op("act", lambda a, vq=vq, pair=pair: a.activation(out=vM3[:, :, vq * 256:(vq + 1) * 256],
                                                                     in_=pair.ap[:, 0:512].rearrange("p (b n) -> p b n", b=2), func=AF.Copy),
                     reads=[pair], writes=[vM])
            P.barrier()
            RB.reset()
            ymem = RY[2].get(8192, BF16)
            ym3 = ymem.ap.rearrange("p (j t) -> p j t", j=8)
            qa = RB.get(2048, BF16)
            qa3 = qa.ap.rearrange("p (d t) -> p d t", d=2)
            Eb = RB.get(2048, BF16)
            Eb3 = Eb.ap.rearrange("p (b t) -> p b t", b=2)
            rden = RB.get(1024, F32)
            for a_ in range(4):
                s = wload("aq", li * 4 + a_, 4096)
                wv = s.ap.rearrange("p (g k m) -> p g k m", g=2, k=KC)
                for d2 in range(2):
                    pair = palloc()
                    mm32(pair, lambda kc, wv=wv, d2=d2: wv[:, d2, kc, :], [s])
                    P.op("act", lambda a, d2=d2, pair=pair: a.activation(out=qa3[:, d2, :], in_=pair.ap, func=AF.Copy), reads=[pair], writes=[qa])
                for mb in range(2):
                    pair = palloc()
                    P.group("pe", [
                        (lambda t, tt=tt, d2=d2, mb=mb, a_=a_, pair=pair: t.matmul(
                            pair.ap[:, tt * 512:(tt + 1) * 512], lhsT=kT3[:, 2 * a_ + d2, mb * 128:(mb + 1) * 128],
                            rhs=qa3[:, d2, tt * 512:(tt + 1) * 512], start=(d2 == 0), stop=(d2 == 1)))
                        for tt in range(2) for d2 in range(2)], reads=[kT, qa], writes=[pair])
                    P.op("act", lambda a, mb=mb, pair=pair: a.activation(out=Eb3[:, mb, :], in_=pair.ap, func=AF.Exp, scale=1.0 / 16.0),
                         reads=[pair], writes=[Eb])
                pair = palloc()
                P.group("pe", [
                    (lambda t, tt=tt, mb=mb, pair=pair: t.matmul(pair.ap[:, tt * 512:(tt + 1) * 512], lhsT=ones_b,
                                                                rhs=Eb3[:, mb, tt * 512:(tt + 1) * 512], start=(mb == 0), stop=(mb == 1)))
                    for tt in range(2) for mb in range(2)], reads=[Eb, CST], writes=[pair])
                P.op("dve", lambda v, pair=pair: v.reciprocal(out=rden.ap, in_=pair.ap), reads=[pair], writes=[rden])
                for dv2 in range(2):
                    pair = palloc()
                    P.group("pe", [
                        (lambda t, tt=tt, mb=mb, a_=a_, dv2=dv2, pair=pair: t.matmul(
                            pair.ap[:, tt * 512:(tt + 1) * 512], lhsT=vM3[:, mb, a_ * 256 + dv2 * 128:a_ * 256 + dv2 * 128 + 128],
                            rhs=Eb3[:, mb, tt * 512:(tt + 1) * 512], start=(mb == 0), stop=(mb == 1)))
                        for tt in range(2) for mb in range(2)], reads=[vM, Eb], writes=[pair])
                    P.op("dve", lambda v, a_=a_, dv2=dv2, pair=pair: v.tensor_tensor(out=ym3[:, 2 * a_ + dv2, :], in0=pair.ap, in1=rden.ap, op=ALU.mult),
                         reads=[pair, rden], writes=[ymem])
            if MIXCUT == 4:
                P.wait("dve", [("cc_r", P.dcnt["cc_r"])])
                reload_x()
                return
            P.barrier()
            RB.reset()
            RY[1].reset()
            yhb = RY[1].get(8192, BF16)
            yh3 = yhb.ap.rearrange("p (h t) -> p h t", h=8)
            FS = [{"sq": RB.get(1024, BF16), "rs": RB.get(1024, F32), "sg": RB.get(1024, F32)} for _ in range(2)]
            ol = [TB(oloc3[:, h_, :]) for h_ in range(8)]
            flag = pcol("flag", 0)
            P.op("dve", lambda v: v.tensor_scalar(out=Sin.ap, in0=recvb.ap[:, 0:1024], scalar1=flag, scalar2=None, op0=ALU.mult), reads=[recvb, par], writes=[Sin])
            P.op("dve", lambda v: v.tensor_scalar(out=uprev, in0=recvb.ap[:, 1024:1152], scalar1=flag, scalar2=None, op0=ALU.mult), reads=[recvb, par], writes=[xsm])

            def fin(h, fs):
                sq, rs, sg = fs["sq"], fs["rs"], fs["sg"]
                pair = palloc()
                P.group("pe", [
                    (lambda t, tt=tt: t.matmul(pair.ap[:, tt * 512:(tt + 1) * 512], lhsT=Sin3[:, h, :],
                                               rhs=Qff3[:, h, tt * 512:(tt + 1) * 512], start=True, stop=True))
                    for tt in range(2)], reads=[Sin, Qff], writes=[pair])
                yield
                P.op("dve", lambda v: v.tensor_tensor(out=ol[h].ap, in0=ol[h].ap, in1=pair.ap, op=ALU.add), reads=[pair, ol[h]], writes=[ol[h]])
                yield
                P.op("act", lambda a: a.activation(out=sq.ap, in_=ol[h].ap, func=AF.Square), reads=[ol[h]], writes=[sq])
                yield
                pair2 = palloc()
                P.group("pe", [
                    (lambda t, tt=tt: t.matmul(pair2.ap[:, tt * 512:(tt + 1) * 512], lhsT=ones_b, rhs=sq.ap[:, tt * 512:(tt + 1) * 512],
                                               start=True, stop=True)) for tt in range(2)], reads=[sq, CST], writes=[pair2])
                yield
                P.op("act", lambda a: a.activation(out=rs.ap, in_=pair2.ap, func=AF.Ln, bias=epsb, scale=1.0 / 128.0), reads=[pair2, CST], writes=[rs])
                yield
                P.op("act", lambda a: a.activation(out=rs.ap, in_=rs.ap, func=AF.Exp, scale=-0.5), reads=[rs], writes=[rs])
                yield
                s_ = wload("hg", li * 8 + h, 2048)
                wv_ = s_.ap[:, 0:2048].rearrange("p (k m) -> p k m", k=KC)
                pg = palloc()
                mm32(pg, lambda kc: wv_[:, kc, :], [s_])
                yield
                P.op("act", lambda a: a.activation(out=sg.ap, in_=pg.ap, func=AF.Silu), reads=[pg], writes=[sg])
                yield
                P.op("dve", lambda v: v.scalar_tensor_tensor(out=ol[h].ap, in0=ol[h].ap, scalar=pcol("hn", l), in1=rs.ap, op0=ALU.mult, op1=ALU.mult),
                     reads=[ol[h], rs, par], writes=[ol[h]])
                yield
                P.op("dve", lambda v: v.tensor_tensor(out=yh3[:, h, :], in0=ol[h].ap, in1=sg.ap, op=ALU.mult), reads=[ol[h], sg], writes=[yhb])
                yield

            for hp_ in range(4):
                drain(fin(2 * hp_, FS[0]), fin(2 * hp_ + 1, FS[1]))
            cw0 = par_t[:, PO["cw"] + l * 24:PO["cw"] + l * 24 + 8]
            cw1 = par_t[:, PO["cw"] + l * 24 + 8:PO["cw"] + l * 24 + 16]
            up3 = uprev.rearrange("p (j k) -> p j k", k=16)
            cp3 = cpre.rearrange("p (j k) -> p j k", k=16)
            cb3 = cbs.rearrange("p (j k) -> p j k", k=16)
            t13 = t1s.rearrange("p (j k) -> p j k", k=2)
            t23 = t2s.rearrange("p (j k) -> p j k", k=2)
            yf3 = yfix.rearrange("p (j k) -> p j k", k=2)
            w0b = cw0.unsqueeze(2).broadcast_to([128, 8, 2])
            w1b = cw1.unsqueeze(2).broadcast_to([128, 8, 2])
            P.op("dve", lambda v: v.tensor_tensor(out=t13, in0=up3[:, :, 14:16], in1=w0b, op=ALU.mult), reads=[xsm, par], writes=[xsm])
            P.op("dve", lambda v: v.tensor_tensor(out=cp3[:, :, 0:2], in0=cp3[:, :, 0:2], in1=t13, op=ALU.add), reads=[xsm], writes=[xsm])
            P.op("dve", lambda v: v.tensor_tensor(out=t23, in0=up3[:, :, 15:16].broadcast_to([128, 8, 2]), in1=w1b, op=ALU.mult), reads=[xsm, par], writes=[xsm])
            P.op("dve", lambda v: v.tensor_tensor(out=t23, in0=t23, in1=mask10, op=ALU.mult), reads=[xsm, CST], writes=[xsm])
            P.op("dve", lambda v: v.tensor_tensor(out=cp3[:, :, 0:2], in0=cp3[:, :, 0:2], in1=t23, op=ALU.add), reads=[xsm], writes=[xsm])
            P.op("dve", lambda v: v.tensor_tensor(out=yf3, in0=cb3[:, :, 0:2], in1=cp3[:, :, 0:2], op=ALU.mult), reads=[xsm], writes=[xsm])
            P.op("dve", lambda v: v.tensor_copy(out=yc3[:, :, 0:2], in_=yf3), reads=[xsm], writes=[yconv])
            reload_x()
            if MIXCUT == 5:
                return
            RB.reset()
            sgm = RB.get(1024, F32)
            macc = RB.get(1024, F32)
            tmp = RB.get(1024, F32)
            mg = RB.get(4096, BF16)
            mg3 = mg.ap.rearrange("p (k t) -> p k t", k=4)
            Ys = [yc3, yh3, ym3]
            Yb = [yconv, yhb, ymem]
            for grp in range(4):
                for d4 in range(4):
                    dc = grp * 4 + d4
                    for n in range(3):
                        s = wload("mg", (li * 16 + dc) * 3 + n, 3072)
                        gw = s.ap[:, 0:2048].rearrange("p (k m) -> p k m", k=KC)
                        bw = s.ap[:, 2048:3072].rearrange("p (k m) -> p k m", k=8)
                        pG = palloc()
                        mm32(pG, lambda kc, gw=gw: gw[:, kc, :], [s])
                        pP = palloc()
                        P.group("pe", [
                            (lambda t, k8=k8, tt=tt, n=n, pP=pP, bw=bw: t.matmul(pP.ap[:, tt * 512:(tt + 1) * 512], lhsT=bw[:, k8, :],
                                                                                rhs=Ys[n][:, k8, tt * 512:(tt + 1) * 512],
                                                                                start=(k8 == 0), stop=(k8 == 7)))
                            for k8 in range(8) for tt in range(2)], reads=[s, Yb[n]], writes=[pP])
                        P.op("act", lambda a, pG=pG: a.activation(out=sgm.ap, in_=pG.ap, func=AF.Sigmoid), reads=[pG], writes=[sgm])
                        if n == 0:
                            P.op("dve", lambda v, pP=pP: v.tensor_tensor(out=macc.ap, in0=sgm.ap, in1=pP.ap, op=ALU.mult), reads=[sgm, pP], writes=[macc])
                        else:
                            P.op("dve", lambda v, pP=pP: v.tensor_tensor(out=tmp.ap, in0=sgm.ap, in1=pP.ap, op=ALU.mult), reads=[sgm, pP], writes=[tmp])
                            if n == 1:
                                P.op("dve", lambda v: v.tensor_tensor(out=macc.ap, in0=macc.ap, in1=tmp.ap, op=ALU.add), reads=[macc, tmp], writes=[macc])
                            else:
                                P.op("dve", lambda v, d4=d4: v.tensor_tensor(out=mg3[:, d4, :], in0=macc.ap, in1=tmp.ap, op=ALU.add), reads=[macc, tmp], writes=[mg])
                for half in range(2):
                    s = wload("wo", (li * 4 + grp) * 2 + half, 4096)
                    wv = s.ap.rearrange("p (d k m) -> p d k m", d=8, k=4)
                    for d8 in range(8):
                        dcq = half * 8 + d8
                        pair = palloc()
                        P.group("pe", [
                            (lambda t, k4=k4, tt=tt, d8=d8, pair=pair, wv=wv: t.matmul(pair.ap[:, tt * 512:(tt + 1) * 512], lhsT=wv[:, d8, k4, :],
                                                                                      rhs=mg3[:, k4, tt * 512:(tt + 1) * 512],
                                                                                      start=(k4 == 0), stop=(k4 == 3)))
                            for k4 in range(4) for tt in range(2)], reads=[s, mg], writes=[pair])
                        P.op("dve", lambda v, dcq=dcq, pair=pair: v.tensor_tensor(out=xT.ap[:, dcq, :], in0=pair.ap, in1=xT.ap[:, dcq, :], op=ALU.add),
                             reads=[pair, xT], writes=[xT])

        for (kind, l) in stages:
            if kind == "ffn1":
                ffn(l, 1)
            elif kind == "ffn2":
                ffn(l, 2)
            else:
                mixer(l)

        RA.reset()
        P.barrier()
        outb = []
        if final_norm:
            rstd = x_rstd(RA)
            ob = RA.get(KC * T, F32)
            ob3 = ob.ap.rearrange("p (c t) -> p c t", c=KC)
            obq = [TB(ob3[:, q * 4:(q + 1) * 4, :]) for q in range(4)]
            for q in range(4):
                for c in range(q * 4, q * 4 + 4):
                    P.op("dve", lambda v, c=c: v.scalar_tensor_tensor(
                        out=ob3[:, c, :], in0=xT.ap[:, c, :], scalar=pcol("nf", c), in1=rstd.ap,
                        op0=ALU.mult, op1=ALU.mult), reads=[xT, rstd, par], writes=[obq[q]])
                tk = P.dma("sp", lambda e, q=q: e.dma_start(
                    out=y_d[q * 512:(q + 1) * 512, :].rearrange("(c p) t -> p c t", p=128),
                    in_=obq[q].ap), "st%d" % q, reads=[obq[q]])
                outb.append(tk)
        else:
            for q in range(4):
                tk = P.dma("sp", lambda e, q=q: e.dma_start(
                    out=y_d[q * 512:(q + 1) * 512, :].rearrange("(c p) t -> p c t", p=128),
                    in_=xT.ap[:, q * 4:(q + 1) * 4, :]), "st%d" % q, reads=[xT])
                outb.append(tk)
        P.wait("sp", outb)

        semkeys = ["pe", "act", "dve", "pool", "sp"] + sorted(P.dcnt.keys())
        sems = {}
        for k in semkeys:
            sems[k] = es.enter_context(nc.semaphore("s_" + k))
        with nc.Block() as block:
            def replay(en, h):
                for o in P.E[en].ops:
                    if o[0] == "wait":
                        h.wait_ge(sems[o[1]], o[2])
                    elif o[0] == "op":
                        ins = o[1](h)
                        if o[2]:
                            ins.then_inc(sems[en], 1)
                    elif o[0] == "custom":
                        ins = o[1](h)
                        ins.then_inc(sems[o[2]], o[3])
                    else:
                        ins = o[1](h)
                        ins.then_inc(sems[o[2]], 16)

            @block.tensor
            def _(t):
                replay("pe", t)

            @block.scalar
            def _(a):
                replay("act", a)

            @block.vector
            def _(v):
                replay("dve", v)

            @block.gpsimd
            def _(g):
                replay("pool", g)

            @block.sync
            def _(s):
                replay("sp", s)
    return nc, sorted(W.keys())


def _lhsT(Wl, c0, ncols=128):
    return Wl[:, c0:c0 + ncols].reshape(KC, 128, ncols).transpose(1, 0, 2)


def prep_weights(inp, layers, used):
    out = {}
    NLL = len(layers)
    ls = list(layers)
    for w in (1, 2):
        if ("gu%d" % w) not in used:
            continue
        Wg = inp["ffn%d_w_gate" % w][ls].reshape(NLL, KC, 128, NF, 128)
        Wu = inp["ffn%d_w_up" % w][ls].reshape(NLL, KC, 128, NF, 128)
        gu = np.empty((NLL, NF, 128, 2, KC, 128), np.float32)
        gu[:, :, :, 0] = Wg.transpose(0, 3, 2, 1, 4)
        gu[:, :, :, 1] = Wu.transpose(0, 3, 2, 1, 4)
        out["w_gu%d" % w] = gu.reshape(NLL * NF, 128, 4096)
        Wd = inp["ffn%d_w_down" % w][ls].reshape(NLL, NG, GJ, 128, 8, 2, 128)
        out["w_d%d" % w] = np.ascontiguousarray(Wd.transpose(0, 1, 4, 3, 5, 2, 6)).reshape(NLL * NG * 8, 128, 2816)
    fam = {k: [] for k in ("qf", "vi", "hg", "cxc", "cb", "aq", "mg", "wo", "kk", "kv")}
    for l in (layers if "qf" in used else []):
        Win = inp["w_in"][l]
        Wkv = inp["w_mem_kv"][l]
        Wb = inp["w_branch"][l]
        Wo = inp["w_o"][l]
        for h in range(8):
            fam["qf"].append(np.stack([_lhsT(Win, 3072 + h * 128), _lhsT(Win, 4096 + h * 128)], 1).reshape(128, 4096))
            fam["hg"].append(_lhsT(Win, 6144 + h * 128).reshape(128, 2048))
            fam["cxc"].append(np.stack([_lhsT(Win, h * 128), _lhsT(Win, 2048 + h * 128)], 1).reshape(128, 4096))
            fam["cb"].append(_lhsT(Win, 1024 + h * 128).reshape(128, 2048))
        for q in range(4):
            fam["vi"].append(_lhsT(Win, 5120 + q * 256, 256).reshape(128, 4096))
            fam["aq"].append(np.stack([_lhsT(Win, 7168 + q * 256), _lhsT(Win, 7168 + q * 256 + 128)], 1).reshape(128, 4096))
            fam["kk"].append(np.stack([_lhsT(Wkv, 2 * q * 128), _lhsT(Wkv, (2 * q + 1) * 128)], 1).reshape(128, 4096))
            fam["kv"].append(_lhsT(Wkv, 1024 + q * 256, 256).reshape(128, 4096))
        for dc in range(16):
            for n in range(3):
                g_ = _lhsT(Win, 8192 + n * 2048 + dc * 128).reshape(128, 2048)
                b_ = Wb[n][:, dc * 128:(dc + 1) * 128].reshape(8, 128, 128).transpose(1, 0, 2).reshape(128, 1024)
                fam["mg"].append(np.concatenate([g_, b_], 1))
        wo = Wo.reshape(4, 4, 128, 2, 8, 128).transpose(0, 3, 2, 4, 1, 5).reshape(8, 128, 4096)
        fam["wo"].extend(list(wo))
    for k, v in fam.items():
        if not v:
            continue
        out["w_" + k] = np.ascontiguousarray(np.stack(v, 0), dtype=np.float32)
    return out


def prep_small(inp, core):
    par = np.zeros((128, NPAR), np.float32)

    def put(name, arr):
        par[:, PO[name]:PO[name] + arr.shape[1]] = arr

    for nm, key in (("n1", "norm_ffn1"), ("nm", "norm_mix"), ("n2", "norm_ffn2"), ("nmem", "mem_norm")):
        put(nm, inp[key].reshape(L, KC, 128).transpose(2, 0, 1).reshape(128, L * KC))
    put("nf", inp["final_norm"].reshape(KC, 128).T)
    put("cw", inp["conv_w"].reshape(L, 3, 8, 128).transpose(3, 0, 1, 2).reshape(128, L * 24))
    put("lbl", inp["hgrn_lb_logits"].reshape(L, 8, 128).transpose(2, 0, 1).reshape(128, L * 8))
    put("hn", inp["hgrn_norm"].T)
    par[:, PO["flag"]] = float(core % 2)
    return par


def make_consts():
    c = np.zeros((128, 2048), np.float32)
    c[:, 0:128] = np.eye(128, dtype=np.float32)
    rm = np.ones((128, 1024), np.float32)
    rm[:, 0::64] = 0.0
    c[:, 128:1152] = rm
    s = np.arange(128)[:, None] % 64
    t = np.arange(64)[None, :]
    c[:, 1152:1664] = np.tile((s <= t).astype(np.float32), (1, 8))
    c[:, 1664:1680] = 1.0
    c[:, 1680:1696] = np.tile(np.array([1.0, 0.0], np.float32), 8)
    return c


STAGES_FULL = [(k, l) for l in range(L) for k in ("ffn1", "mix", "ffn2")]
_cache = {}


def run(inp, stages, final_norm=True, trace=False):
    key = (tuple(stages), final_norm)
    if key not in _cache:
        _cache[key] = build(stages, final_norm)
    nc, used = _cache[key]
    x = np.asarray(inp["x"], np.float32)
    mem = np.asarray(inp["mem"], np.float32)
    import time as _t
    _t0 = _t.time()
    layers = sorted({l for _, l in stages})
    wts = prep_weights(inp, layers, used)
    print("[kernel] host prep %.1fs" % (_t.time() - _t0), flush=True)
    cst = make_consts()
    in_maps = []
    for c in range(8):
        b, h = c // 2, c % 2
        m = {"xT": np.ascontiguousarray(x[b, h * T:(h + 1) * T, :].T),
             "memT": np.ascontiguousarray(mem[b].T),
             "par": prep_small(inp, c), "cst": cst}
        m.update({("w_" + k): wts["w_" + k] for k in used})
        in_maps.append(m)
    res = run_bass_kernel_spmd(nc, in_maps, core_ids=list(range(8)), trace=trace)
    out = np.empty((4, 2048, D), np.float32)
    for c in range(8):
        b, h = c // 2, c % 2
        out[b, h * T:(h + 1) * T, :] = res.results[c]["yT"].T
    return out, res


def kernel(**inputs):
    inp = {k: np.asarray(v) for k, v in inputs.items()}
    out, _ = run(inp, STAGES_FULL, True)
    return out
```
